# Optimizing a Trainium2 kernel written in Bass

```python
import jax, jax.numpy as jnp
from jax import lax
import numpy as np

D_MODEL = 2048
BATCH = 8
SEQ = 2048
DEPTH = 4

CHUNK = 64
D_RNN = D_MODEL // 2
RG_HEADS = 16
RG_HEAD_DIM = D_RNN // RG_HEADS
RG_CONV = 4
RG_C = 8.0
D_CONV = D_MODEL // 2
SC_WIDTH = 3
D_FF = 3 * D_MODEL
N_EXPERTS = 8
TOP_K = 2
D_FF_EXPERT = 3 * D_MODEL // 2
N_MOD = 6
EPS = 1e-6
IN_SPLITS = (D_RNN, 2 * D_RNN, 2 * D_RNN + D_CONV, 2 * D_RNN + 2 * D_CONV,
             2 * D_RNN + 3 * D_CONV, 2 * D_RNN + 3 * D_CONV + D_MODEL)
D_IN = 2 * D_RNN + 3 * D_CONV + 2 * D_MODEL

kernel_name = "hybrid_rglru_shortconv_moe_adaln"


def _rmsnorm(x, g):
    xf = x.astype(jnp.float32)
    y = xf * lax.rsqrt(jnp.mean(xf * xf, axis=-1, keepdims=True) + EPS)
    return (y * g.astype(jnp.float32)).astype(x.dtype)


def _modulate(h, shift, scale):
    return h * (1 + scale[:, None, :]) + shift[:, None, :]


def _causal_dwconv(x, w, b):
    k = w.shape[0]
    s = x.shape[1]
    xp = jnp.pad(x, ((0, 0), (k - 1, 0), (0, 0)))
    y = b
    for j in range(k):
        y = y + xp[:, j:j + s, :] * w[j]
    return y


def _scan_combine(left, right):
    a_l, b_l = left
    a_r, b_r = right
    return a_l * a_r, a_r * b_l + b_r


def _rg_lru(xc, r, i, lam):
    bsz, s, w = xc.shape
    nc = s // CHUNK
    log_a = -RG_C * r.astype(jnp.float32) * jax.nn.softplus(-lam.astype(jnp.float32))
    a = jnp.exp(log_a)
    u = jnp.sqrt(-jnp.expm1(2.0 * log_a)) * (i.astype(jnp.float32) * xc.astype(jnp.float32))
    a = a.reshape(bsz, nc, CHUNK, w)
    u = u.reshape(bsz, nc, CHUNK, w)
    a_cum, h_loc = lax.associative_scan(_scan_combine, (a, u), axis=2)

    def carry_step(h_prev, chunk_end):
        a_end, h_end = chunk_end
        return a_end * h_prev + h_end, h_prev

    _, h_in = lax.scan(carry_step, jnp.zeros((bsz, w), jnp.float32),
                       (jnp.moveaxis(a_cum[:, :, -1], 1, 0), jnp.moveaxis(h_loc[:, :, -1], 1, 0)))
    h = h_loc + a_cum * jnp.moveaxis(h_in, 0, 1)[:, :, None, :]
    return h.reshape(bsz, s, w).astype(xc.dtype)


def _hybrid_mixer(h, w_in, conv_a_w, conv_a_b, w_rg_a, b_rg_a, w_rg_x, b_rg_x, rg_lambda,
                  w_out_a, conv_b_w, conv_b_b, w_out_b, w_o):
    z = h @ w_in
    a_x, a_g, b_v, b_bg, b_cg, g_a, g_b = jnp.split(z, IN_SPLITS, axis=-1)
    bsz, s, _ = h.shape
    xc = _causal_dwconv(a_x, conv_a_w, conv_a_b)
    xh = xc.reshape(bsz, s, RG_HEADS, RG_HEAD_DIM)
    r = jax.nn.sigmoid(jnp.einsum('bshd,hde->bshe', xh, w_rg_a).reshape(bsz, s, D_RNN) + b_rg_a)
    i = jax.nn.sigmoid(jnp.einsum('bshd,hde->bshe', xh, w_rg_x).reshape(bsz, s, D_RNN) + b_rg_x)
    hr = _rg_lru(xc, r, i, rg_lambda)
    y_a = (jax.nn.gelu(a_g, approximate=True) * hr) @ w_out_a
    y_b = (b_bg * _causal_dwconv(b_cg * b_v, conv_b_w, conv_b_b)) @ w_out_b
    merged = jax.nn.sigmoid(g_a) * y_a + jax.nn.sigmoid(g_b) * y_b
    return merged @ w_o


def _swiglu(h, wg, wu, wd):
    return (jax.nn.silu(h @ wg) * (h @ wu)) @ wd


def _moe(h, w_router, b_router, w_e_gate, w_e_up, w_e_down):
    bsz, s, d = h.shape
    t = h.reshape(bsz * s, d)
    logits = (t @ w_router).astype(jnp.float32) + b_router.astype(jnp.float32)
    top_vals, top_idx = lax.top_k(logits, TOP_K)
    top_w = jax.nn.softmax(top_vals, axis=-1)
    gates = jnp.einsum('nk,nke->ne', top_w,
                       jax.nn.one_hot(top_idx, N_EXPERTS, dtype=jnp.float32)).astype(h.dtype)
    out = jnp.zeros_like(t)
    for e in range(N_EXPERTS):
        out = out + gates[:, e:e + 1] * _swiglu(t, w_e_gate[e], w_e_up[e], w_e_down[e])
    return out.reshape(bsz, s, d)


def setup_inputs(seed: int = 0) -> dict:
    key = jax.random.key(seed)
    ks = jax.random.split(key, 32)
    f32 = jnp.float32
    n_dense = (DEPTH + 1) // 2
    n_moe = DEPTH // 2

    def nrm(k, shape, scale):
        return jax.random.normal(k, shape, f32) * scale

    a_c = jax.random.uniform(ks[13], (DEPTH, D_RNN), f32, minval=0.9, maxval=0.999)
    a0 = a_c ** (1.0 / RG_C)
    rg_lambda = jnp.log(a0) - jnp.log1p(-a0)
    return {
        "x": nrm(ks[0], (BATCH, SEQ, D_MODEL), 1.0),
        "c": nrm(ks[1], (BATCH, D_MODEL), 1.0),
        "w_mod": nrm(ks[2], (DEPTH, D_MODEL, N_MOD * D_MODEL), 0.5 * D_MODEL ** -0.5),
        "b_mod": nrm(ks[3], (DEPTH, N_MOD * D_MODEL), 0.02),
        "norm1_g": 1.0 + nrm(ks[4], (DEPTH, D_MODEL), 0.02),
        "norm2_g": 1.0 + nrm(ks[5], (DEPTH, D_MODEL), 0.02),
        "w_in": nrm(ks[6], (DEPTH, D_MODEL, D_IN), D_MODEL ** -0.5),
        "conv_a_w": nrm(ks[7], (DEPTH, RG_CONV, D_RNN), RG_CONV ** -0.5),
        "conv_a_b": nrm(ks[8], (DEPTH, D_RNN), 0.02),
        "w_rg_a": nrm(ks[9], (DEPTH, RG_HEADS, RG_HEAD_DIM, RG_HEAD_DIM), RG_HEAD_DIM ** -0.5),
        "b_rg_a": nrm(ks[10], (DEPTH, D_RNN), 0.02),
        "w_rg_x": nrm(ks[11], (DEPTH, RG_HEADS, RG_HEAD_DIM, RG_HEAD_DIM), RG_HEAD_DIM ** -0.5),
        "b_rg_x": nrm(ks[12], (DEPTH, D_RNN), 0.02),
        "rg_lambda": rg_lambda,
        "w_out_a": nrm(ks[14], (DEPTH, D_RNN, D_MODEL), D_RNN ** -0.5),
        "conv_b_w": nrm(ks[15], (DEPTH, SC_WIDTH, D_CONV), SC_WIDTH ** -0.5),
        "conv_b_b": nrm(ks[16], (DEPTH, D_CONV), 0.02),
        "w_out_b": nrm(ks[17], (DEPTH, D_CONV, D_MODEL), D_CONV ** -0.5),
        "w_o": nrm(ks[18], (DEPTH, D_MODEL, D_MODEL), D_MODEL ** -0.5),
        "w_ff_gate": nrm(ks[19], (n_dense, D_MODEL, D_FF), D_MODEL ** -0.5),
        "w_ff_up": nrm(ks[20], (n_dense, D_MODEL, D_FF), D_MODEL ** -0.5),
        "w_ff_down": nrm(ks[21], (n_dense, D_FF, D_MODEL), D_FF ** -0.5),
        "w_router": nrm(ks[22], (n_moe, D_MODEL, N_EXPERTS), D_MODEL ** -0.5),
        "b_router": nrm(ks[23], (n_moe, N_EXPERTS), 0.01),
        "w_e_gate": nrm(ks[24], (n_moe, N_EXPERTS, D_MODEL, D_FF_EXPERT), D_MODEL ** -0.5),
        "w_e_up": nrm(ks[25], (n_moe, N_EXPERTS, D_MODEL, D_FF_EXPERT), D_MODEL ** -0.5),
        "w_e_down": nrm(ks[26], (n_moe, N_EXPERTS, D_FF_EXPERT, D_MODEL), D_FF_EXPERT ** -0.5),
        "final_g": 1.0 + nrm(ks[27], (D_MODEL,), 0.02),
    }


def reference(x, c, w_mod, b_mod, norm1_g, norm2_g, w_in, conv_a_w, conv_a_b, w_rg_a, b_rg_a,
              w_rg_x, b_rg_x, rg_lambda, w_out_a, conv_b_w, conv_b_b, w_out_b, w_o,
              w_ff_gate, w_ff_up, w_ff_down, w_router, b_router, w_e_gate, w_e_up, w_e_down,
              final_g):
    c_act = jax.nn.silu(c)
    for l in range(DEPTH):
        mod = c_act @ w_mod[l] + b_mod[l]
        sh1, sc1, g1, sh2, sc2, g2 = jnp.split(mod, N_MOD, axis=-1)
        h = _modulate(_rmsnorm(x, norm1_g[l]), sh1, sc1)
        y = _hybrid_mixer(h, w_in[l], conv_a_w[l], conv_a_b[l], w_rg_a[l], b_rg_a[l],
                          w_rg_x[l], b_rg_x[l], rg_lambda[l], w_out_a[l], conv_b_w[l],
                          conv_b_b[l], w_out_b[l], w_o[l])
        x = x + g1[:, None, :] * y
        h = _modulate(_rmsnorm(x, norm2_g[l]), sh2, sc2)
        j = l // 2
        if l % 2 == 0:
            f = _swiglu(h, w_ff_gate[j], w_ff_up[j], w_ff_down[j])
        else:
            f = _moe(h, w_router[j], b_router[j], w_e_gate[j], w_e_up[j], w_e_down[j])
        x = x + g2[:, None, :] * f
    return _rmsnorm(x, final_g)
```

```python
from contextlib import ExitStack
import numpy as np
import concourse.bass as bass
import concourse.mybir as mybir
from concourse.bass_utils import run_bass_kernel_spmd

F32 = mybir.dt.float32
BF16 = mybir.dt.bfloat16
AF = mybir.ActivationFunctionType
ALU = mybir.AluOpType
AX = mybir.AxisListType

ENGS = ("tensor", "vector", "scalar", "gpsimd", "sync")
D = 2048
KC = 16
T = 512
DR = 1024
NCH = 8
EPS = 1e-6
EPOCH = 16000
DIN = 9216


class Ev:
    __slots__ = ("eng", "sem", "val", "signaled", "clock")

    def __init__(self, eng):
        self.eng = eng
        self.sem = None
        self.val = None
        self.signaled = False
        self.clock = None


class Rec:
    def __init__(self):
        self.q = {e: [] for e in ENGS}
        self.last_w = {}
        self.readers = {}
        self.dma_cnt = {}
        self.dma_ep = {}
        self.alias = {}
        self.order = []

    def _deps(self, reads, writes):
        deps = []
        for k in reads:
            w = self.last_w.get(k)
            if w is not None:
                deps.append(w)
        for k in writes:
            w = self.last_w.get(k)
            if w is not None:
                deps.append(w)
            deps.extend(self.readers.get(k, ()))
        return deps

    def _commit(self, ev, reads, writes):
        for k in reads:
            self.readers.setdefault(k, []).append(ev)
        for k in writes:
            self.last_w[k] = ev
            self.readers[k] = []

    def _canon(self, keys):
        a = self.alias
        return [a.get(k, k) for k in keys]

    def op(self, eng, fn, reads=(), writes=()):
        reads = self._canon(reads)
        writes = self._canon(writes)
        deps = self._deps(reads, writes)
        ev = Ev(eng)
        for d in deps:
            d.signaled = True
        self.order.append((eng, len(self.q[eng])))
        self.q[eng].append(("op", fn, ev, deps))
        self._commit(ev, reads, writes)
        return ev

    def dma(self, eng, fn, semname, reads=(), writes=()):
        reads = self._canon(reads)
        writes = self._canon(writes)
        deps = self._deps(reads, writes)
        ev = Ev(eng)
        ev.signaled = True
        ep = self.dma_ep.get(semname, 0)
        if self.dma_cnt.get((semname, ep), 0) + 16 > EPOCH:
            ep += 1
            self.dma_ep[semname] = ep
        self.dma_cnt[(semname, ep)] = self.dma_cnt.get((semname, ep), 0) + 16
        ev.sem = (semname, ep)
        ev.val = self.dma_cnt[(semname, ep)]
        for d in deps:
            d.signaled = True
        self.order.append((eng, len(self.q[eng])))
        self.q[eng].append(("dma", fn, ev, deps))
        self._commit(ev, reads, writes)
        return ev

    def wait_all(self, eng, keys):
        keys = self._canon(keys)
        deps = [self.last_w[k] for k in keys if k in self.last_w]
        for d in deps:
            d.signaled = True
        self.order.append((eng, len(self.q[eng])))
        self.q[eng].append(("wait", None, None, deps))

    def emit(self, nc, stack):
        prog = {}
        dsem = {n: stack.enter_context(nc.semaphore("dma_%s_%d" % n)) for n in self.dma_cnt}
        for e in ENGS:
            n = 0
            for kind, fn, ev, deps in self.q[e]:
                if kind == "op" and ev.signaled:
                    k = n // EPOCH
                    n += 1
                    if (e, k) not in prog:
                        prog[(e, k)] = stack.enter_context(nc.semaphore("prog_%s_%d" % (e, k)))
                    ev.sem = prog[(e, k)]
                    ev.val = n - k * EPOCH
                elif kind == "dma":
                    ev.sem = dsem[ev.sem]
        clock = {e: {} for e in ENGS}
        waits = {e: [None] * len(self.q[e]) for e in ENGS}
        for e, idx in self.order:
            kind, fn, ev, deps = self.q[e][idx]
            ck = clock[e]
            need = {}
            for d in deps:
                key = id(d.sem)
                if ck.get(key, 0) < d.val and need.get(key, (None, 0))[1] < d.val:
                    need[key] = (d.sem, d.val)
            for d in deps:
                for k2, v2 in d.clock.items():
                    if ck.get(k2, 0) < v2:
                        ck[k2] = v2
                key = id(d.sem)
                if ck.get(key, 0) < d.val:
                    ck[key] = d.val
            waits[e][idx] = list(need.values())
            if ev is not None and (kind == "dma" or ev.signaled):
                c2 = dict(ck)
                c2[id(ev.sem)] = max(c2.get(id(ev.sem), 0), ev.val)
                ev.clock = c2
        block = stack.enter_context(nc.Block())
        stats = {}

        def run(e):
            def body(engine):
                nw = 0
                for idx, (kind, fn, ev, deps) in enumerate(self.q[e]):
                    for sem, val in waits[e][idx]:
                        engine.wait_ge(sem, val)
                        nw += 1
                    if kind == "wait":
                        continue
                    ins = fn(engine)
                    if kind == "dma":
                        ins.then_inc(ev.sem, 16)
                    elif ev.signaled:
                        ins.then_inc(ev.sem, 1)
                stats[e] = (len(self.q[e]), nw)
            return body

        block.tensor(run("tensor"))
        block.vector(run("vector"))
        block.scalar(run("scalar"))
        block.gpsimd(run("gpsimd"))
        block.sync(run("sync"))
        return stats


def pp_layout(NB, L, LM):
    off = {}
    n = 0
    for name, w in (("c", NB * 16), ("bmod", L * 96), ("n1g", L * 16), ("n2g", L * 16), ("fg", 16),
                    ("caw", L * 32), ("cab", L * 8), ("bra", L * 8), ("brx", L * 8), ("lam", L * 8),
                    ("cbw", L * 24), ("cbb", L * 8), ("brt", max(LM, 1) * 8)):
        off[name] = n
        n += w
    return off, n


def build(cfg):
    NB, S, L = cfg["NB"], cfg["S"], cfg["L"]
    DFF, DFFE, NE = cfg["DFF"], cfg["DFFE"], cfg["NE"]
    LD, LM = (L + 1) // 2, L // 2
    NTILE = S // T
    assert NTILE % 2 == 0
    NSLOT = 4
    SLOTE = 4096
    PO, NPP = pp_layout(NB, L, LM)
    NPW = 2 * NCH * 128 + KC * 8
    SKIP = cfg.get("skip", ())

    nc = bass.Bass("TRN2", target_bir_lowering=False)
    dt = lambda name, shape, kind="ExternalInput": nc.dram_tensor(name, shape, F32, kind=kind).ap()
    x_d = dt("x", [NB * S, D])
    pp_d = dt("pp", [128, NPP])
    pw_d = dt("pw", [L * 128, NPW])
    id_d = dt("ident", [128, 128])
    wmod_d = dt("w_mod", [L * D, 6 * D])
    win_d = dt("w_in", [L * D, DIN])
    woa_d = dt("w_out_a", [L * DR, D])
    wob_d = dt("w_out_b", [L * DR, D])
    wo_d = dt("w_o", [L * D, D])
    wfg_d = dt("w_ff_gate", [LD * D, DFF])
    wfu_d = dt("w_ff_up", [LD * D, DFF])
    wfd_d = dt("w_ff_down", [LD * DFF, D])
    weg_d = dt("w_e_gate", [max(LM, 1) * NE * D, DFFE])
    weu_d = dt("w_e_up", [max(LM, 1) * NE * D, DFFE])
    wed_d = dt("w_e_down", [max(LM, 1) * NE * DFFE, D])
    y_d = dt("y", [NB * S, D], kind="ExternalOutput")

    R = Rec()
    R.alias = {"t_s2": "t_gl", "t_v2": "t_m", "t_s1": "t_u", "xtok0": "t_r", "xtok1": "t_i"}
    st = ExitStack()
    sb = lambda name, shape, dtype=F32: st.enter_context(nc.sbuf_tensor(name, shape, dtype))
    xs2 = [sb("xs%d" % i, [128, KC, T]) for i in range(2)]
    hT2 = [sb("hT%d" % i, [128, KC, T], BF16) for i in range(2)]
    pAB = sb("pAB", [128, 16, T], BF16)
    mg = sb("mg", [128, KC, T], BF16)
    wsl = [sb("wsl%d" % i, [128, SLOTE], BF16) for i in range(NSLOT)]
    gbcj = sb("gbcj", [128, 2, 2, T], BF16)
    ppt = sb("ppt", [128, NPP])
    pwt = sb("pwt", [128, NPW], BF16)
    modv = sb("modv", [128, NB, L * 96])
    Amod = sb("Amod", [128, NB, L * 32])
    c1t = sb("c1t", [128, L * 8])
    cact = sb("cact", [128, KC, NB], BF16)
    ident_f = sb("ident_f", [128, 128])
    ones_f = sb("ones_f", [128, 128])
    ones_b = sb("ones_b", [128, 128], BF16)
    cst = sb("cst", [128, 2])
    sA = sb("sA", [128, L * NCH, 3])
    sBt = sb("sBt", [128, L * NCH, 2])
    sH = sb("sH", [128, L * NCH])
    axbuf = [sb("axbuf%d" % i, [128, T + 3]) for i in range(2)]
    cvbuf = sb("cvbuf", [128, T + 2])
    tnames = ("t_xc", "t_r", "t_i", "t_a", "t_m", "t_u", "t_h", "t_gl", "t_v1", "rstd")
    tm = {n: sb(n, [128, T]) for n in tnames}
    for a_, c_ in R.alias.items():
        if a_.startswith("t_"):
            tm[a_] = tm[c_]
    xtok = [tm["t_r"], tm["t_i"]]
    xcb = sb("xcb", [128, T], BF16)
    lg = sb("lg", [128, 2, 4, 8])
    gate = sb("gate", [128, 2, 4, 8])
    m8 = sb("m8", [128, 8])
    sm = sb("sm", [128, 8])
    sc4 = sb("sc4", [128, 4])
    dg = [sb("dg%d" % i, [128, 128]) for i in range(2)]
    ps = st.enter_context(nc.psum_tensor("ps", [128, 8, 512], F32))

    bank_ctr = [0]

    def nbank():
        b = bank_ctr[0] % 8
        bank_ctr[0] += 1
        return b

    slot_ctr = [0]

    def wtile(src, kcn, nw):
        s = slot_ctr[0] % NSLOT
        slot_ctr[0] += 1
        view = wsl[s][:, 0:kcn * nw].rearrange("p (k n) -> p k n", n=nw)
        srcv = src.rearrange("(k p) n -> p k n", p=128)
        R.dma("gpsimd", lambda e: e.dma_start(out=view, in_=srcv), "w%d" % s, writes=[("ws", s)])
        return view, ("ws", s)

    def mmg(bank, pairs, reads, ncols=512, c0=0):
        n = len(pairs)

        def fn(e):
            ins = None
            for i, (l, r) in enumerate(pairs):
                ins = e.matmul(ps[:, bank, c0:c0 + ncols], lhsT=l, rhs=r, start=(i == 0), stop=(i == n - 1))
            return ins
        return R.op("tensor", fn, reads=reads, writes=[("ps", bank)])

    def act(out, in_, func, reads, writes, bias=None, scale=None):
        kw = {}
        if bias is not None:
            kw["bias"] = bias
        if scale is not None:
            kw["scale"] = scale
        return R.op("scalar", lambda e: e.activation(out=out, in_=in_, func=func, **kw), reads=reads, writes=writes)

    def vtt(out, in0, in1, op, reads, writes):
        return R.op("vector", lambda e: e.tensor_tensor(out=out, in0=in0, in1=in1, op=op), reads=reads, writes=writes)

    def vts(out, in0, s1, s2, op0, op1, reads, writes):
        if s2 is None:
            return R.op("vector", lambda e: e.tensor_scalar(out=out, in0=in0, scalar1=s1, scalar2=None, op0=op0),
                        reads=reads, writes=writes)
        return R.op("vector", lambda e: e.tensor_scalar(out=out, in0=in0, scalar1=s1, scalar2=s2, op0=op0, op1=op1),
                    reads=reads, writes=writes)

    def vstt(out, in0, scalar, in1, op0, op1, reads, writes):
        return R.op("vector", lambda e: e.scalar_tensor_tensor(out=out, in0=in0, scalar=scalar, in1=in1, op0=op0, op1=op1),
                    reads=reads, writes=writes)

    def col(name, i):
        o = PO[name] + i
        return ppt[:, o:o + 1]

    R.dma("sync", lambda e: e.dma_start(out=ppt[:], in_=pp_d[:, :]), "pp", writes=["ppt"])
    R.dma("sync", lambda e: e.dma_start(out=ident_f[:], in_=id_d[:, :]), "id", writes=["ident"])
    R.op("vector", lambda e: e.memset(ones_f[:], 1.0), writes=["ones_f"])
    R.op("vector", lambda e: e.memset(ones_b[:], 1.0), writes=["ones_b"])
    R.op("vector", lambda e: e.memset(cst[:, 0:1], EPS), writes=["cst0"])
    R.op("vector", lambda e: e.memset(cst[:, 1:2], 1.0), writes=["cst"])
    for b in range(NB):
        act(cact[:, :, b], ppt[:, PO["c"] + b * 16:PO["c"] + (b + 1) * 16], AF.Silu, ["ppt"], [("cact", b)])
    lam = ppt[:, PO["lam"]:PO["lam"] + L * 8]
    act(c1t[:], lam, AF.Exp, ["ppt"], ["c1a"], scale=-1.0)
    act(c1t[:], c1t[:], AF.Ln, ["c1a", "cst"], ["c1b"], bias=cst[:, 1:2])
    vts(c1t[:], c1t[:], -8.0, None, ALU.mult, None, ["c1b"], ["c1"])
    def mod_gen(l):
        for jb in range(24 if "mod" not in SKIP else 0):
            w0, k0 = wtile(wmod_d[l * D:l * D + 1024, jb * 512:(jb + 1) * 512], 8, 512)
            w1, k1 = wtile(wmod_d[l * D + 1024:(l + 1) * D, jb * 512:(jb + 1) * 512], 8, 512)
            for j4 in range(4):
                v16 = jb * 4 + j4
                bk = nbank()
                pairs = []
                for kc in range(KC):
                    w = w0 if kc < 8 else w1
                    pairs.append((w[:, kc % 8, j4 * 128:(j4 + 1) * 128], cact[:, kc, :]))
                mmg(bk, pairs, [k0, k1] + [("cact", b) for b in range(NB)], ncols=NB)
                vts(modv[:, :, l * 96 + v16], ps[:, bk, 0:NB], col("bmod", l * 96 + v16), None, ALU.add, None,
                    [("ps", bk), "ppt"], [("modv", l, v16)])
            yield
        for b in range(NB if "mod" not in SKIP else 0):
            for which in range(2):
                gname = "n1g" if which == 0 else "n2g"
                vstt(Amod[:, b, l * 32 + which * 16:l * 32 + which * 16 + 16],
                     modv[:, b, l * 96 + which * 48 + 16:l * 96 + which * 48 + 32], 1.0,
                     ppt[:, PO[gname] + l * 16:PO[gname] + (l + 1) * 16], ALU.add, ALU.mult,
                     [("modv", l, which * 48 + 16 + i) for i in range(16)] + ["ppt"], [("Amod", b, l, which)])
        yield

    for _ in mod_gen(0):
        pass
    bg_gen = [None]

    def modcol(b, l, v, j):
        o = l * 96 + v * 16 + j
        return modv[:, b, o:o + 1]

    def hkeys(par):
        return [("hT", par, kc) for kc in range(KC)]

    def norm_to_hT(b, l, which, par):
        xs, hT = xs2[par], hT2[par]
        for kc in range(KC):
            if kc % 2 == 0:
                act(hT[:, kc, :], xs[:, kc, :], AF.Square, [("xs", par, kc)], [("hT", par, kc)])
            else:
                vtt(hT[:, kc, :], xs[:, kc, :], xs[:, kc, :], ALU.mult, [("xs", par, kc)], [("hT", par, kc)])
        bk = nbank()
        mmg(bk, [(ones_b[:], hT[:, kc, :]) for kc in range(KC)], ["ones_b"] + hkeys(par))
        act(tm["t_s1"][:], ps[:, bk, :], AF.Sqrt, [("ps", bk), "cst0"], ["t_s1"], bias=cst[:, 0:1], scale=1.0 / D)
        R.op("vector", lambda e: e.reciprocal(out=tm["rstd"][:], in_=tm["t_s1"][:]), reads=["t_s1"], writes=["rstd"])
        if which == 2:
            return
        aoff = l * 32 + which * 16
        for kc in range(KC):
            tn = "t_v1" if kc % 2 == 0 else "t_v2"
            vtt(tm[tn][:], xs[:, kc, :], tm["rstd"][:], ALU.mult, [("xs", par, kc), "rstd"], [tn])
            act(hT[:, kc, :], tm[tn][:], AF.Identity, [tn, ("Amod", b, l, which), ("modv", l, which * 48 + kc)],
                [("hT", par, kc)], bias=modcol(b, l, which * 3, kc), scale=Amod[:, b, aoff + kc:aoff + kc + 1])

    def win_tiles(l, c0, nw):
        if nw <= 256:
            w, k = wtile(win_d[l * D:(l + 1) * D, c0:c0 + nw], 16, nw)
            return (lambda kc, a, bb: w[:, kc, a:bb]), [k]
        w0, k0 = wtile(win_d[l * D:l * D + 1024, c0:c0 + nw], 8, nw)
        w1, k1 = wtile(win_d[l * D + 1024:(l + 1) * D, c0:c0 + nw], 8, nw)
        return (lambda kc, a, bb: (w0 if kc < 8 else w1)[:, kc % 8, a:bb]), [k0, k1]

    def mixer(b, l, ti, par):
        xs, hT = xs2[par], hT2[par]
        hT_all = hkeys(par)
        norm_to_hT(b, l, 0, par)
        R.dma("gpsimd", lambda e: e.dma_start(out=pwt[:], in_=pw_d[l * 128:(l + 1) * 128, :]), "pw", writes=["pwt"])

        def ZA(c):
            gx, kx = win_tiles(l, c * 128, 128)
            gg, kg = win_tiles(l, DR + c * 128, 128)
            b1 = nbank()
            mmg(b1, [(gx(kc, 0, 128), hT[:, kc, :]) for kc in range(KC)], kx + hT_all)
            b2 = nbank()
            mmg(b2, [(gg(kc, 0, 128), hT[:, kc, :]) for kc in range(KC)], kg + hT_all)
            return b1, b2

        nxt = ZA(0)
        for c in range(NCH):
            bax, bag = nxt
            if c + 1 < NCH:
                nxt = ZA(c + 1)
            lc = l * NCH + c
            ab = axbuf[c % 2]
            abk = "axbuf%d" % (c % 2)
            if ti == 0:
                R.op("vector", lambda e, ab=ab: e.memset(ab[:, 0:3], 0.0), writes=[abk + "h"])
            else:
                R.op("vector", lambda e, ab=ab, lc=lc: e.tensor_copy(out=ab[:, 0:3], in_=sA[:, lc, :]),
                     reads=[("sA", lc)], writes=[abk + "h"])
            act(ab[:, 3:T + 3], ps[:, bax, :], AF.Copy, [("ps", bax)], [abk])
            R.op("vector", lambda e, ab=ab, lc=lc: e.tensor_copy(out=sA[:, lc, :], in_=ab[:, T:T + 3]),
                 reads=[abk], writes=[("sA", lc)])
            xc = tm["t_xc"]
            vts(xc[:], ab[:, 0:T], col("caw", lc * 4 + 0), col("cab", lc), ALU.mult, ALU.add, [abk, abk + "h", "ppt"], ["t_xc"])
            for j in range(1, 4):
                vstt(xc[:], ab[:, j:j + T], col("caw", lc * 4 + j), xc[:], ALU.mult, ALU.add,
                     [abk, abk + "h", "t_xc", "ppt"], ["t_xc"])
            act(xcb[:], xc[:], AF.Copy, ["t_xc"], ["xcb"])
            br = nbank()
            mmg(br, [(pwt[:, c * 128:(c + 1) * 128], xcb[:])], ["pwt", "xcb"])
            bi = nbank()
            mmg(bi, [(pwt[:, (NCH + c) * 128:(NCH + c + 1) * 128], xcb[:])], ["pwt", "xcb"])
            act(tm["t_r"][:], ps[:, br, :], AF.Sigmoid, [("ps", br), "ppt"], ["t_r"], bias=col("bra", lc))
            act(tm["t_i"][:], ps[:, bi, :], AF.Sigmoid, [("ps", bi), "ppt"], ["t_i"], bias=col("brx", lc))
            act(tm["t_a"][:], tm["t_r"][:], AF.Exp, ["t_r", "c1"], ["t_a"], scale=c1t[:, lc:lc + 1])
            act(tm["t_m"][:], tm["t_a"][:], AF.Square, ["t_a"], ["t_m"])
            act(tm["t_m"][:], tm["t_m"][:], AF.Sqrt, ["t_m", "cst"], ["t_m"], bias=cst[:, 1:2], scale=-1.0)
            vtt(tm["t_u"][:], tm["t_i"][:], xc[:], ALU.mult, ["t_i", "t_xc"], ["t_u"])
            vtt(tm["t_u"][:], tm["t_u"][:], tm["t_m"][:], ALU.mult, ["t_u", "t_m"], ["t_u"])
            if ti == 0:
                R.op("vector", lambda e: e.tensor_tensor_scan(out=tm["t_h"][:], data0=tm["t_a"][:], data1=tm["t_u"][:],
                                                              initial=0.0, op0=ALU.mult, op1=ALU.add),
                     reads=["t_a", "t_u"], writes=["t_h"])
            else:
                R.op("vector", lambda e, lc=lc: e.tensor_tensor_scan(out=tm["t_h"][:], data0=tm["t_a"][:], data1=tm["t_u"][:],
                                                                     initial=sH[:, lc:lc + 1], op0=ALU.mult, op1=ALU.add),
                     reads=["t_a", "t_u", ("sH", lc)], writes=["t_h"])
            R.op("vector", lambda e, lc=lc: e.tensor_copy(out=sH[:, lc:lc + 1], in_=tm["t_h"][:, T - 1:T]),
                 reads=["t_h"], writes=[("sH", lc)])
            act(tm["t_gl"][:], ps[:, bag, :], AF.Gelu_apprx_tanh, [("ps", bag)], ["t_gl"])
            vtt(pAB[:, c, :], tm["t_gl"][:], tm["t_h"][:], ALU.mult, ["t_gl", "t_h"], [("pab", c)])

        def ZB(c):
            out = []
            for r in range(3):
                g_, k_ = win_tiles(l, 2 * DR + r * DR + c * 128, 128)
                bk = nbank()
                mmg(bk, [(g_(kc, 0, 128), hT[:, kc, :]) for kc in range(KC)], k_ + hT_all)
                out.append(bk)
            return out

        nxt = ZB(0)
        for c in range(NCH):
            bv, bbg, bcg = nxt
            if c + 1 < NCH:
                nxt = ZB(c + 1)
            lc = l * NCH + c
            if ti == 0:
                R.op("vector", lambda e: e.memset(cvbuf[:, 0:2], 0.0), writes=["cvh"])
            else:
                R.op("vector", lambda e, lc=lc: e.tensor_copy(out=cvbuf[:, 0:2], in_=sBt[:, lc, :]),
                     reads=[("sB", lc)], writes=["cvh"])
            act(tm["t_s1"][:], ps[:, bcg, :], AF.Copy, [("ps", bcg)], ["t_s1"])
            vtt(cvbuf[:, 2:T + 2], tm["t_s1"][:], ps[:, bv, :], ALU.mult, ["t_s1", ("ps", bv)], ["cv"])
            R.op("vector", lambda e, lc=lc: e.tensor_copy(out=sBt[:, lc, :], in_=cvbuf[:, T:T + 2]),
                 reads=["cv"], writes=[("sB", lc)])
            cvo = tm["t_v1"]
            vts(cvo[:], cvbuf[:, 0:T], col("cbw", lc * 3 + 0), col("cbb", lc), ALU.mult, ALU.add, ["cv", "cvh", "ppt"], ["t_v1"])
            for j in range(1, 3):
                vstt(cvo[:], cvbuf[:, j:j + T], col("cbw", lc * 3 + j), cvo[:], ALU.mult, ALU.add,
                     ["cv", "cvh", "t_v1", "ppt"], ["t_v1"])
            vtt(pAB[:, 8 + c, :], cvo[:], ps[:, bbg, :], ALU.mult, ["t_v1", ("ps", bbg)], [("pab", 8 + c)])

        pA_all = [("pab", c) for c in range(NCH)]
        pB_all = [("pab", 8 + c) for c in range(NCH)]
        for jp in range(8):
            cs = slice(jp * 256, (jp + 1) * 256)
            j0, j1 = jp * 2, jp * 2 + 1
            sl0, sl1 = slice(0, 128), slice(128, 256)
            woa, ka = wtile(woa_d[l * DR:(l + 1) * DR, cs], 8, 256)
            bya0 = nbank()
            mmg(bya0, [(woa[:, c, sl0], pAB[:, c, :]) for c in range(NCH)], [ka] + pA_all)
            bya1 = nbank()
            mmg(bya1, [(woa[:, c, sl1], pAB[:, c, :]) for c in range(NCH)], [ka] + pA_all)
            gga, kga = win_tiles(l, 5 * DR + jp * 256, 256)
            bga0 = nbank()
            mmg(bga0, [(gga(kc, 0, 128), hT[:, kc, :]) for kc in range(KC)], kga + hT_all)
            bga1 = nbank()
            mmg(bga1, [(gga(kc, 128, 256), hT[:, kc, :]) for kc in range(KC)], kga + hT_all)
            act(tm["t_r"][:], ps[:, bga0, :], AF.Sigmoid, [("ps", bga0)], ["t_r"])
            vtt(tm["t_a"][:], tm["t_r"][:], ps[:, bya0, :], ALU.mult, ["t_r", ("ps", bya0)], ["t_a"])
            act(tm["t_i"][:], ps[:, bga1, :], AF.Sigmoid, [("ps", bga1)], ["t_i"])
            vtt(tm["t_u"][:], tm["t_i"][:], ps[:, bya1, :], ALU.mult, ["t_i", ("ps", bya1)], ["t_u"])
            wob, kb = wtile(wob_d[l * DR:(l + 1) * DR, cs], 8, 256)
            byb0 = nbank()
            mmg(byb0, [(wob[:, c, sl0], pAB[:, 8 + c, :]) for c in range(NCH)], [kb] + pB_all)
            byb1 = nbank()
            mmg(byb1, [(wob[:, c, sl1], pAB[:, 8 + c, :]) for c in range(NCH)], [kb] + pB_all)
            ggb, kgb = win_tiles(l, 5 * DR + D + jp * 256, 256)
            bgb0 = nbank()
            mmg(bgb0, [(ggb(kc, 0, 128), hT[:, kc, :]) for kc in range(KC)], kgb + hT_all)
            bgb1 = nbank()
            mmg(bgb1, [(ggb(kc, 128, 256), hT[:, kc, :]) for kc in range(KC)], kgb + hT_all)
            act(tm["t_xc"][:], ps[:, bgb0, :], AF.Sigmoid, [("ps", bgb0)], ["t_xc"])
            vtt(tm["t_h"][:], tm["t_xc"][:], ps[:, byb0, :], ALU.mult, ["t_xc", ("ps", byb0)], ["t_h"])
            vtt(mg[:, j0, :], tm["t_a"][:], tm["t_h"][:], ALU.add, ["t_a", "t_h"], [("mg", j0)])
            act(tm["t_gl"][:], ps[:, bgb1, :], AF.Sigmoid, [("ps", bgb1)], ["t_gl"])
            vtt(tm["t_v1"][:], tm["t_gl"][:], ps[:, byb1, :], ALU.mult, ["t_gl", ("ps", byb1)], ["t_v1"])
            vtt(mg[:, j1, :], tm["t_u"][:], tm["t_v1"][:], ALU.add, ["t_u", "t_v1"], [("mg", j1)])
        mg_all = [("mg", j) for j in range(KC)]
        for jg in range(4):
            w0, k0 = wtile(wo_d[l * D:l * D + 1024, jg * 512:(jg + 1) * 512], 8, 512)
            w1, k1 = wtile(wo_d[l * D + 1024:(l + 1) * D, jg * 512:(jg + 1) * 512], 8, 512)
            for j4 in range(4):
                j = jg * 4 + j4
                sl = slice(j4 * 128, (j4 + 1) * 128)
                bo = nbank()
                mmg(bo, [((w0 if k < 8 else w1)[:, k % 8, sl], mg[:, k, :]) for k in range(KC)], [k0, k1] + mg_all)
                vstt(xs[:, j, :], ps[:, bo, :], modcol(b, l, 2, j), xs[:, j, :], ALU.mult, ALU.add,
                     [("ps", bo), ("modv", l, 32 + j), ("xs", par, j)], [("xs", par, j)])

    def emit_gbc(e_):
        buf = e_ % 2
        for par in range(2):
            bk = nbank()
            for s in range(4):
                d_ = dg[s % 2]
                dk = "dg%d" % (s % 2)
                vts(d_[:], ident_f[:], gate[:, par, s, e_:e_ + 1], None, ALU.mult, None, ["ident", ("gate", par, s)], [dk])
                R.op("tensor", lambda e, d_=d_, s=s, bk=bk: e.matmul(ps[:, bk, s * 128:(s + 1) * 128], lhsT=ones_f[:], rhs=d_[:],
                                                                      start=True, stop=True),
                     reads=[dk, "ones_f"], writes=[("ps", bk)] if s == 0 else [("psx", bk, s)])
            act(gbcj[:, buf, par, :], ps[:, bk, :], AF.Copy, [("ps", bk)] + [("psx", bk, s) for s in range(1, 4)],
                [("gbc", buf, par)])

    def ffn_groups(b, l, groups):
        seen = set()

        def GU(gi):
            gsrc, usrc, dsrc, e_ = groups[gi]
            if e_ is not None and e_ not in seen:
                seen.add(e_)
                emit_gbc(e_)
            for half in range(2):
                gt, kg = wtile(gsrc(half), 16, 256)
                ut, ku = wtile(usrc(half), 16, 256)
                for q2 in range(2):
                    q = half * 2 + q2
                    sl = slice(q2 * 128, (q2 + 1) * 128)
                    for par in range(2):
                        hT = hT2[par]
                        bg = nbank()
                        mmg(bg, [(gt[:, kc, sl], hT[:, kc, :]) for kc in range(KC)], [kg] + hkeys(par))
                        bu = nbank()
                        mmg(bu, [(ut[:, kc, sl], hT[:, kc, :]) for kc in range(KC)], [ku] + hkeys(par))
                        ch = (gi % 2) * 8 + par * 4 + q
                        act(tm["t_s2"][:], ps[:, bg, :], AF.Silu, [("ps", bg)], ["t_s2"])
                        if e_ is not None:
                            vtt(tm["t_s2"][:], tm["t_s2"][:], gbcj[:, e_ % 2, par, :], ALU.mult,
                                ["t_s2", ("gbc", e_ % 2, par)], ["t_s2"])
                        vtt(pAB[:, ch, :], tm["t_s2"][:], ps[:, bu, :], ALU.mult, ["t_s2", ("ps", bu)], [("pab", ch)])

        def DN(gi):
            gsrc, usrc, dsrc, e_ = groups[gi]
            d0, kd0 = wtile(dsrc(0), 4, 1024)
            d1, kd1 = wtile(dsrc(1), 4, 1024)
            for j in range(KC):
                dd, kd = (d0, kd0) if j < 8 else (d1, kd1)
                sl = slice((j % 8) * 128, (j % 8 + 1) * 128)
                for par in range(2):
                    base = (gi % 2) * 8 + par * 4
                    bd = nbank()
                    mmg(bd, [(dd[:, k, sl], pAB[:, base + k, :]) for k in range(4)],
                        [kd] + [("pab", base + k) for k in range(4)])
                    vstt(xs2[par][:, j, :], ps[:, bd, :], modcol(b, l, 5, j), xs2[par][:, j, :], ALU.mult, ALU.add,
                         [("ps", bd), ("modv", l, 80 + j), ("xs", par, j)], [("xs", par, j)])

        GU(0)
        per = -(-26 // len(groups))
        for gi in range(len(groups)):
            if gi + 1 < len(groups):
                GU(gi + 1)
            DN(gi)
            if bg_gen[0] is not None:
                for _ in range(per):
                    if next(bg_gen[0], "end") == "end":
                        bg_gen[0] = None
                        break
        if bg_gen[0] is not None:
            for _ in bg_gen[0]:
                pass
            bg_gen[0] = None

    def ffn_dense(b, l):
        ld = l // 2
        norm_to_hT(b, l, 1, 1)
        groups = []
        for g in range(DFF // 512):
            groups.append((
                lambda half, g=g: wfg_d[ld * D:(ld + 1) * D, g * 512 + half * 256:g * 512 + (half + 1) * 256],
                lambda half, g=g: wfu_d[ld * D:(ld + 1) * D, g * 512 + half * 256:g * 512 + (half + 1) * 256],
                lambda hh, g=g: wfd_d[ld * DFF + g * 512:ld * DFF + (g + 1) * 512, hh * 1024:(hh + 1) * 1024],
                None))
        ffn_groups(b, l, groups)

    def ffn_moe(b, l):
        lm = l // 2
        ro = 2 * NCH * 128
        for par in range(2):
            if par == 1:
                norm_to_hT(b, l, 1, par)
            hT = hT2[par]
            for s in range(4):
                bk = nbank()
                mmg(bk, [(hT[:, kc, s * 128:(s + 1) * 128], pwt[:, ro + kc * 8:ro + (kc + 1) * 8]) for kc in range(KC)],
                    ["pwt"] + hkeys(par), ncols=8)
                gk = ("gate", par, s)
                lk = ("lg", par, s)
                vtt(lg[:, par, s, :], ps[:, bk, 0:8], ppt[:, PO["brt"] + lm * 8:PO["brt"] + (lm + 1) * 8], ALU.add,
                    [("ps", bk), "ppt"], [lk])
                R.op("vector", lambda e, s=s, par=par: e.max(out=m8[:], in_=lg[:, par, s, :]), reads=[lk], writes=["m8"])
                vts(sm[:], lg[:, par, s, :], m8[:, 1:2], None, ALU.is_ge, None, [lk, "m8"], ["sm"])
                vts(sc4[:, 0:1], m8[:, 0:1], -1.0, None, ALU.mult, None, ["m8"], ["m8n"])
                act(gate[:, par, s, :], lg[:, par, s, :], AF.Exp, [lk, "m8n"], [gk], bias=sc4[:, 0:1])
                vtt(gate[:, par, s, :], gate[:, par, s, :], sm[:], ALU.mult, [gk, "sm"], [gk])
                R.op("vector", lambda e, s=s, par=par: e.tensor_reduce(out=sc4[:, 1:2], in_=gate[:, par, s, :], axis=AX.X,
                                                                       op=ALU.add), reads=[gk], writes=["den"])
                R.op("vector", lambda e: e.reciprocal(out=sc4[:, 2:3], in_=sc4[:, 1:2]), reads=["den"], writes=["rden"])
                vts(gate[:, par, s, :], gate[:, par, s, :], sc4[:, 2:3], None, ALU.mult, None, [gk, "rden"], [gk])
        groups = []
        for e_ in range(NE):
            base = (lm * NE + e_) * D
            dbase = (lm * NE + e_) * DFFE
            for g in range(DFFE // 512):
                groups.append((
                    lambda half, g=g, base=base: weg_d[base:base + D, g * 512 + half * 256:g * 512 + (half + 1) * 256],
                    lambda half, g=g, base=base: weu_d[base:base + D, g * 512 + half * 256:g * 512 + (half + 1) * 256],
                    lambda hh, g=g, dbase=dbase: wed_d[dbase + g * 512:dbase + (g + 1) * 512, hh * 1024:(hh + 1) * 1024],
                    e_))
        ffn_groups(b, l, groups)

    n = 0
    for b in range(NB):
        for pair in range(NTILE // 2):
            for par in range(2):
                ti = pair * 2 + par
                r0 = b * S + ti * T
                xs = xs2[par]
                for s in range(4):
                    for q in range(4):
                        xt = xtok[n % 2]
                        xk = "xtok%d" % (n % 2)
                        R.dma("sync", lambda e, xt=xt, s=s, q=q, r0=r0: e.dma_start(
                            out=xt[:], in_=x_d[r0 + s * 128:r0 + (s + 1) * 128, q * 512:(q + 1) * 512]), xk, writes=[xk])
                        bk = nbank()

                        def tp(e, xt=xt, bk=bk):
                            ins = None
                            for i in range(4):
                                ins = e.transpose(out=ps[:, bk, i * 128:(i + 1) * 128], in_=xt[:, i * 128:(i + 1) * 128],
                                                  identity=ident_f[:])
                            return ins
                        R.op("tensor", tp, reads=[xk, "ident"], writes=[("ps", bk)])
                        src3 = ps[:, bk, :].rearrange("p (i c) -> p i c", c=128)
                        dst3 = xs[:, q * 4:(q + 1) * 4, s * 128:(s + 1) * 128]
                        wk = [("xs", par, q * 4 + i) for i in range(4)]
                        if n % 2 == 0:
                            act(dst3, src3, AF.Copy, [("ps", bk)], wk)
                        else:
                            R.op("vector", lambda e, dst3=dst3, src3=src3: e.tensor_copy(out=dst3, in_=src3),
                                 reads=[("ps", bk)], writes=wk)
                        n += 1
            for l in range(L):
                if "mixer" not in SKIP:
                    mixer(b, l, pair * 2, 0)
                if "ffn" not in SKIP:
                    norm_to_hT(b, l, 1, 0)
                if "mixer" not in SKIP:
                    mixer(b, l, pair * 2 + 1, 1)
                if "ffn" in SKIP:
                    continue
                if b == 0 and pair == 0 and l + 1 < L:
                    bg_gen[0] = mod_gen(l + 1)
                if l % 2 == 0:
                    ffn_dense(b, l)
                else:
                    ffn_moe(b, l)
            for par in range(2):
                ti = pair * 2 + par
                r0 = b * S + ti * T
                xs = xs2[par]
                norm_to_hT(b, 0, 2, par)
                for kc in range(KC):
                    vstt(xs[:, kc, :], xs[:, kc, :], col("fg", kc), tm["rstd"][:], ALU.mult, ALU.mult,
                         [("xs", par, kc), "rstd", "ppt"], [("xs", par, kc)])
                for s in range(4):
                    for q in range(4):
                        bk = nbank()

                        def tp2(e, s=s, q=q, bk=bk, xs=xs):
                            ins = None
                            for i in range(4):
                                ins = e.transpose(out=ps[:, bk, i * 128:(i + 1) * 128],
                                                  in_=xs[:, q * 4 + i, s * 128:(s + 1) * 128], identity=ident_f[:])
                            return ins
                        R.op("tensor", tp2, reads=[("xs", par, q * 4 + i) for i in range(4)] + ["ident"], writes=[("ps", bk)])
                        xt = xtok[n % 2]
                        xk = "xtok%d" % (n % 2)
                        n += 1
                        act(xt[:], ps[:, bk, :], AF.Copy, [("ps", bk)], [xk])
                        R.dma("sync", lambda e, xt=xt, s=s, q=q, r0=r0: e.dma_start(
                            out=y_d[r0 + s * 128:r0 + (s + 1) * 128, q * 512:(q + 1) * 512], in_=xt[:]), "o" + xk,
                            reads=[xk], writes=["out_" + xk])
    R.wait_all("sync", ["out_xtok0", "out_xtok1"])
    stats = R.emit(nc, st)
    st.close()
    return nc, stats


def host_pack(inp, cfg, core, ncores):
    NB, S, L = cfg["NB"], cfg["S"], cfg["L"]
    NE = cfg["NE"]
    LM = L // 2
    PO, NPP = pp_layout(NB, L, LM)
    pp = np.zeros((128, NPP), np.float32)
    f = lambda a: np.asarray(a, np.float32)
    c = f(inp["c"])[core * NB:(core + 1) * NB]
    pp[:, PO["c"]:PO["c"] + NB * 16] = c.reshape(NB, 16, 128).transpose(2, 0, 1).reshape(128, NB * 16)
    pp[:, PO["bmod"]:PO["bmod"] + L * 96] = f(inp["b_mod"])[:L].reshape(L, 96, 128).transpose(2, 0, 1).reshape(128, L * 96)
    pp[:, PO["n1g"]:PO["n1g"] + L * 16] = f(inp["norm1_g"])[:L].reshape(L, 16, 128).transpose(2, 0, 1).reshape(128, L * 16)
    pp[:, PO["n2g"]:PO["n2g"] + L * 16] = f(inp["norm2_g"])[:L].reshape(L, 16, 128).transpose(2, 0, 1).reshape(128, L * 16)
    pp[:, PO["fg"]:PO["fg"] + 16] = f(inp["final_g"]).reshape(16, 128).T
    pp[:, PO["caw"]:PO["caw"] + L * 32] = f(inp["conv_a_w"])[:L].reshape(L, 4, 8, 128).transpose(3, 0, 2, 1).reshape(128, L * 32)
    for name, key in (("cab", "conv_a_b"), ("bra", "b_rg_a"), ("brx", "b_rg_x"), ("lam", "rg_lambda"), ("cbb", "conv_b_b")):
        pp[:, PO[name]:PO[name] + L * 8] = f(inp[key])[:L].reshape(L, 8, 128).transpose(2, 0, 1).reshape(128, L * 8)
    pp[:, PO["cbw"]:PO["cbw"] + L * 24] = f(inp["conv_b_w"])[:L].reshape(L, 3, 8, 128).transpose(3, 0, 2, 1).reshape(128, L * 24)
    if LM > 0:
        pp[:, PO["brt"]:PO["brt"] + LM * 8] = np.broadcast_to(f(inp["b_router"])[:LM].reshape(1, LM * 8), (128, LM * 8))
    NPW = 2 * NCH * 128 + KC * 8
    pw = np.zeros((L, 128, NPW), np.float32)
    for gi, key in enumerate(("w_rg_a", "w_rg_x")):
        w = f(inp[key])[:L]
        for c_ in range(NCH):
            for hh in range(2):
                pw[:, hh * 64:(hh + 1) * 64, (gi * NCH + c_) * 128 + hh * 64:(gi * NCH + c_) * 128 + (hh + 1) * 64] = w[:, 2 * c_ + hh]
    for l in range(L):
        if l % 2 == 1:
            wr = f(inp["w_router"])[l // 2]
            pw[l, :, 2 * NCH * 128:] = wr.reshape(16, 128, NE).transpose(1, 0, 2).reshape(128, 16 * NE)
    return pp, pw.reshape(L * 128, NPW)


FULL = dict(NB=1, S=2048, L=4, DFF=6144, DFFE=3072, NE=8)
NCORES = 8
_cache = {}


def run(inp, cfg, ncores):
    NB, S, L = cfg["NB"], cfg["S"], cfg["L"]
    LD, LM = (L + 1) // 2, L // 2
    key = tuple(sorted((k, v) for k, v in cfg.items()))
    if key not in _cache:
        _cache[key] = build(cfg)
    nc, stats = _cache[key]
    f = lambda a: np.ascontiguousarray(np.asarray(a, np.float32))
    shared = {
        "ident": np.eye(128, dtype=np.float32),
        "w_mod": f(inp["w_mod"])[:L].reshape(L * D, 6 * D),
        "w_in": f(inp["w_in"])[:L].reshape(L * D, DIN),
        "w_out_a": f(inp["w_out_a"])[:L].reshape(L * DR, D),
        "w_out_b": f(inp["w_out_b"])[:L].reshape(L * DR, D),
        "w_o": f(inp["w_o"])[:L].reshape(L * D, D),
        "w_ff_gate": f(inp["w_ff_gate"])[:LD].reshape(LD * D, cfg["DFF"]),
        "w_ff_up": f(inp["w_ff_up"])[:LD].reshape(LD * D, cfg["DFF"]),
        "w_ff_down": f(inp["w_ff_down"])[:LD].reshape(LD * cfg["DFF"], D),
        "w_e_gate": f(inp["w_e_gate"])[:max(LM, 1)].reshape(-1, cfg["DFFE"]),
        "w_e_up": f(inp["w_e_up"])[:max(LM, 1)].reshape(-1, cfg["DFFE"]),
        "w_e_down": f(inp["w_e_down"])[:max(LM, 1)].reshape(-1, D),
    }
    x = f(inp["x"])
    in_maps = []
    for core in range(ncores):
        pp, pw = host_pack(inp, cfg, core, ncores)
        m = dict(shared)
        m["x"] = x[core * NB:(core + 1) * NB].reshape(NB * S, D)
        m["pp"] = pp
        m["pw"] = pw
        in_maps.append(m)
    res = run_bass_kernel_spmd(nc, in_maps, core_ids=list(range(ncores)))
    out = np.concatenate([r["y"].reshape(NB, S, D) for r in res.results], axis=0)
    return out.astype(np.float32)


def kernel(**inputs):
    return run(inputs, FULL, NCORES)
```

```python
from contextlib import ExitStack
import numpy as np
import concourse.bass as bass
import concourse.mybir as mybir
from concourse.bass_utils import run_bass_kernel_spmd

F32 = mybir.dt.float32
BF16 = mybir.dt.bfloat16
AF = mybir.ActivationFunctionType
ALU = mybir.AluOpType
AX = mybir.AxisListType

ENGS = ("tensor", "vector", "scalar", "gpsimd", "sync")
D = 2048
KC = 16
T = 512
DR = 1024
NCH = 8
EPS = 1e-6
EPOCH = 16000
DIN = 9216


class Ev:
    __slots__ = ("eng", "sem", "val", "signaled", "clock")

    def __init__(self, eng):
        self.eng = eng
        self.sem = None
        self.val = None
        self.signaled = False
        self.clock = None


class Rec:
    def __init__(self):
        self.q = {e: [] for e in ENGS}
        self.last_w = {}
        self.readers = {}
        self.dma_cnt = {}
        self.dma_ep = {}
        self.alias = {}
        self.order = []

    def _deps(self, reads, writes):
        deps = []
        for k in reads:
            w = self.last_w.get(k)
            if w is not None:
                deps.append(w)
        for k in writes:
            w = self.last_w.get(k)
            if w is not None:
                deps.append(w)
            deps.extend(self.readers.get(k, ()))
        return deps

    def _commit(self, ev, reads, writes):
        for k in reads:
            self.readers.setdefault(k, []).append(ev)
        for k in writes:
            self.last_w[k] = ev
            self.readers[k] = []

    def _canon(self, keys):
        a = self.alias
        return [a.get(k, k) for k in keys]

    def op(self, eng, fn, reads=(), writes=()):
        reads = self._canon(reads)
        writes = self._canon(writes)
        deps = self._deps(reads, writes)
        ev = Ev(eng)
        for d in deps:
            d.signaled = True
        self.order.append((eng, len(self.q[eng])))
        self.q[eng].append(("op", fn, ev, deps))
        self._commit(ev, reads, writes)
        return ev

    def dma(self, eng, fn, semname, reads=(), writes=()):
        reads = self._canon(reads)
        writes = self._canon(writes)
        deps = self._deps(reads, writes)
        ev = Ev(eng)
        ev.signaled = True
        ep = self.dma_ep.get(semname, 0)
        if self.dma_cnt.get((semname, ep), 0) + 16 > EPOCH:
            ep += 1
            self.dma_ep[semname] = ep
        self.dma_cnt[(semname, ep)] = self.dma_cnt.get((semname, ep), 0) + 16
        ev.sem = (semname, ep)
        ev.val = self.dma_cnt[(semname, ep)]
        for d in deps:
            d.signaled = True
        self.order.append((eng, len(self.q[eng])))
        self.q[eng].append(("dma", fn, ev, deps))
        self._commit(ev, reads, writes)
        return ev

    def wait_all(self, eng, keys):
        keys = self._canon(keys)
        deps = [self.last_w[k] for k in keys if k in self.last_w]
        for d in deps:
            d.signaled = True
        self.order.append((eng, len(self.q[eng])))
        self.q[eng].append(("wait", None, None, deps))

    def emit(self, nc, stack):
        prog = {}
        dsem = {n: stack.enter_context(nc.semaphore("dma_%s_%d" % n)) for n in self.dma_cnt}
        for e in ENGS:
            n = 0
            for kind, fn, ev, deps in self.q[e]:
                if kind == "op" and ev.signaled:
                    k = n // EPOCH
                    n += 1
                    if (e, k) not in prog:
                        prog[(e, k)] = stack.enter_context(nc.semaphore("prog_%s_%d" % (e, k)))
                    ev.sem = prog[(e, k)]
                    ev.val = n - k * EPOCH
                elif kind == "dma":
                    ev.sem = dsem[ev.sem]
        clock = {e: {} for e in ENGS}
        waits = {e: [None] * len(self.q[e]) for e in ENGS}
        for e, idx in self.order:
            kind, fn, ev, deps = self.q[e][idx]
            ck = clock[e]
            need = {}
            for d in deps:
                key = id(d.sem)
                if ck.get(key, 0) < d.val and need.get(key, (None, 0))[1] < d.val:
                    need[key] = (d.sem, d.val)
            for d in deps:
                for k2, v2 in d.clock.items():
                    if ck.get(k2, 0) < v2:
                        ck[k2] = v2
                key = id(d.sem)
                if ck.get(key, 0) < d.val:
                    ck[key] = d.val
            waits[e][idx] = list(need.values())
            if ev is not None and (kind == "dma" or ev.signaled):
                c2 = dict(ck)
                c2[id(ev.sem)] = max(c2.get(id(ev.sem), 0), ev.val)
                ev.clock = c2
        block = stack.enter_context(nc.Block())
        stats = {}

        def run(e):
            def body(engine):
                nw = 0
                for idx, (kind, fn, ev, deps) in enumerate(self.q[e]):
                    for sem, val in waits[e][idx]:
                        engine.wait_ge(sem, val)
                        nw += 1
                    if kind == "wait":
                        continue
                    ins = fn(engine)
                    if kind == "dma":
                        ins.then_inc(ev.sem, 16)
                    elif ev.signaled:
                        ins.then_inc(ev.sem, 1)
                stats[e] = (len(self.q[e]), nw)
            return body

        block.tensor(run("tensor"))
        block.vector(run("vector"))
        block.scalar(run("scalar"))
        block.gpsimd(run("gpsimd"))
        block.sync(run("sync"))
        return stats


def pp_layout(NB, L, LM):
    off = {}
    n = 0
    for name, w in (("c", NB * 16), ("bmod", L * 96), ("n1g", L * 16), ("n2g", L * 16), ("fg", 16),
                    ("caw", L * 32), ("cab", L * 8), ("bra", L * 8), ("brx", L * 8), ("lam", L * 8),
                    ("cbw", L * 24), ("cbb", L * 8), ("brt", max(LM, 1) * 8)):
        off[name] = n
        n += w
    return off, n


def build(cfg):
    NB, S, L = cfg["NB"], cfg["S"], cfg["L"]
    DFF, DFFE, NE = cfg["DFF"], cfg["DFFE"], cfg["NE"]
    LD, LM = (L + 1) // 2, L // 2
    NTILE = S // T
    assert NTILE % 2 == 0
    NSLOT = 4
    SLOTE = 4096
    PO, NPP = pp_layout(NB, L, LM)
    NPW = 2 * NCH * 128 + KC * 8
    SKIP = cfg.get("skip", ())

    nc = bass.Bass("TRN2", target_bir_lowering=False)
    dt = lambda name, shape, kind="ExternalInput": nc.dram_tensor(name, shape, F32, kind=kind).ap()
    x_d = dt("x", [NB * S, D])
    pp_d = dt("pp", [128, NPP])
    pw_d = dt("pw", [L * 128, NPW])
    id_d = dt("ident", [128, 128])
    wmod_d = dt("w_mod", [L * D, 6 * D])
    win_d = dt("w_in", [L * D, DIN])
    woa_d = dt("w_out_a", [L * DR, D])
    wob_d = dt("w_out_b", [L * DR, D])
    wo_d = dt("w_o", [L * D, D])
    wfg_d = dt("w_ff_gate", [LD * D, DFF])
    wfu_d = dt("w_ff_up", [LD * D, DFF])
    wfd_d = dt("w_ff_down", [LD * DFF, D])
    weg_d = dt("w_e_gate", [max(LM, 1) * NE * D, DFFE])
    weu_d = dt("w_e_up", [max(LM, 1) * NE * D, DFFE])
    wed_d = dt("w_e_down", [max(LM, 1) * NE * DFFE, D])
    y_d = dt("y", [NB * S, D], kind="ExternalOutput")

    R = Rec()
    R.alias = {"t_s2": "t_gl", "t_v2": "t_m", "t_s1": "t_u", "xtok0": "t_r", "xtok1": "t_i"}
    st = ExitStack()
    sb = lambda name, shape, dtype=F32: st.enter_context(nc.sbuf_tensor(name, shape, dtype))
    xs2 = [sb("xs%d" % i, [128, KC, T]) for i in range(2)]
    hT2 = [sb("hT%d" % i, [128, KC, T], BF16) for i in range(2)]
    pAB = sb("pAB", [128, 16, T], BF16)
    mg = sb("mg", [128, KC, T], BF16)
    wsl = [sb("wsl%d" % i, [128, SLOTE], BF16) for i in range(NSLOT)]
    gbcj = sb("gbcj", [128, 2, 2, T], BF16)
    ppt = sb("ppt", [128, NPP])
    pwt = sb("pwt", [128, NPW], BF16)
    modv = sb("modv", [128, NB, L * 96])
    Amod = sb("Amod", [128, NB, L * 32])
    c1t = sb("c1t", [128, L * 8])
    cact = sb("cact", [128, KC, NB], BF16)
    ident_f = sb("ident_f", [128, 128])
    ones_f = sb("ones_f", [128, 128])
    ones_b = sb("ones_b", [128, 128], BF16)
    cst = sb("cst", [128, 2])
    sA = sb("sA", [128, L * NCH, 3])
    sBt = sb("sBt", [128, L * NCH, 2])
    sH = sb("sH", [128, L * NCH])
    axbuf = [sb("axbuf%d" % i, [128, T + 3]) for i in range(2)]
    cvbuf = sb("cvbuf", [128, T + 2])
    tnames = ("t_xc", "t_r", "t_i", "t_a", "t_m", "t_u", "t_h", "t_gl", "t_v1", "rstd")
    tm = {n: sb(n, [128, T]) for n in tnames}
    for a_, c_ in R.alias.items():
        if a_.startswith("t_"):
            tm[a_] = tm[c_]
    xtok = [tm["t_r"], tm["t_i"]]
    xcb = sb("xcb", [128, T], BF16)
    lg = sb("lg", [128, 2, 4, 8])
    gate = sb("gate", [128, 2, 4, 8])
    m8 = sb("m8", [128, 8])
    sm = sb("sm", [128, 8])
    sc4 = sb("sc4", [128, 4])
    dg = [sb("dg%d" % i, [128, 128]) for i in range(2)]
    ps = st.enter_context(nc.psum_tensor("ps", [128, 8, 512], F32))

    bank_ctr = [0]

    def nbank():
        b = bank_ctr[0] % 8
        bank_ctr[0] += 1
        if R.last_w.get(("ps", b)) is not None and not R.readers.get(("ps", b)):
            raise RuntimeError("PSUM bank %d re-allocated before its evacuation was recorded" % b)
        return b

    slot_ctr = [0]

    def wtile(src, kcn, nw):
        s = slot_ctr[0] % NSLOT
        slot_ctr[0] += 1
        view = wsl[s][:, 0:kcn * nw].rearrange("p (k n) -> p k n", n=nw)
        srcv = src.rearrange("(k p) n -> p k n", p=128)
        R.dma("gpsimd", lambda e: e.dma_start(out=view, in_=srcv), "w%d" % s, writes=[("ws", s)])
        return view, ("ws", s)

    def mmg(bank, pairs, reads, ncols=512, c0=0):
        n = len(pairs)

        def fn(e):
            ins = None
            for i, (l, r) in enumerate(pairs):
                ins = e.matmul(ps[:, bank, c0:c0 + ncols], lhsT=l, rhs=r, start=(i == 0), stop=(i == n - 1))
            return ins
        return R.op("tensor", fn, reads=reads, writes=[("ps", bank)])

    def act(out, in_, func, reads, writes, bias=None, scale=None):
        kw = {}
        if bias is not None:
            kw["bias"] = bias
        if scale is not None:
            kw["scale"] = scale
        return R.op("scalar", lambda e: e.activation(out=out, in_=in_, func=func, **kw), reads=reads, writes=writes)

    def vtt(out, in0, in1, op, reads, writes):
        return R.op("vector", lambda e: e.tensor_tensor(out=out, in0=in0, in1=in1, op=op), reads=reads, writes=writes)

    def vts(out, in0, s1, s2, op0, op1, reads, writes):
        if s2 is None:
            return R.op("vector", lambda e: e.tensor_scalar(out=out, in0=in0, scalar1=s1, scalar2=None, op0=op0),
                        reads=reads, writes=writes)
        return R.op("vector", lambda e: e.tensor_scalar(out=out, in0=in0, scalar1=s1, scalar2=s2, op0=op0, op1=op1),
                    reads=reads, writes=writes)

    def vstt(out, in0, scalar, in1, op0, op1, reads, writes):
        return R.op("vector", lambda e: e.scalar_tensor_tensor(out=out, in0=in0, scalar=scalar, in1=in1, op0=op0, op1=op1),
                    reads=reads, writes=writes)

    def col(name, i):
        o = PO[name] + i
        return ppt[:, o:o + 1]

    R.dma("sync", lambda e: e.dma_start(out=ppt[:], in_=pp_d[:, :]), "pp", writes=["ppt"])
    R.dma("sync", lambda e: e.dma_start(out=ident_f[:], in_=id_d[:, :]), "id", writes=["ident"])
    R.op("vector", lambda e: e.memset(ones_f[:], 1.0), writes=["ones_f"])
    R.op("vector", lambda e: e.memset(ones_b[:], 1.0), writes=["ones_b"])
    R.op("vector", lambda e: e.memset(cst[:, 0:1], EPS), writes=["cst0"])
    R.op("vector", lambda e: e.memset(cst[:, 1:2], 1.0), writes=["cst"])
    for b in range(NB):
        act(cact[:, :, b], ppt[:, PO["c"] + b * 16:PO["c"] + (b + 1) * 16], AF.Silu, ["ppt"], [("cact", b)])
    lam = ppt[:, PO["lam"]:PO["lam"] + L * 8]
    act(c1t[:], lam, AF.Exp, ["ppt"], ["c1a"], scale=-1.0)
    act(c1t[:], c1t[:], AF.Ln, ["c1a", "cst"], ["c1b"], bias=cst[:, 1:2])
    vts(c1t[:], c1t[:], -8.0, None, ALU.mult, None, ["c1b"], ["c1"])
    def mod_gen(l):
        for jb in range(24 if "mod" not in SKIP else 0):
            w0, k0 = wtile(wmod_d[l * D:l * D + 1024, jb * 512:(jb + 1) * 512], 8, 512)
            w1, k1 = wtile(wmod_d[l * D + 1024:(l + 1) * D, jb * 512:(jb + 1) * 512], 8, 512)
            for j4 in range(4):
                v16 = jb * 4 + j4
                bk = nbank()
                pairs = []
                for kc in range(KC):
                    w = w0 if kc < 8 else w1
                    pairs.append((w[:, kc % 8, j4 * 128:(j4 + 1) * 128], cact[:, kc, :]))
                mmg(bk, pairs, [k0, k1] + [("cact", b) for b in range(NB)], ncols=NB)
                vts(modv[:, :, l * 96 + v16], ps[:, bk, 0:NB], col("bmod", l * 96 + v16), None, ALU.add, None,
                    [("ps", bk), "ppt"], [("modv", l, v16)])
            yield
        for b in range(NB if "mod" not in SKIP else 0):
            for which in range(2):
                gname = "n1g" if which == 0 else "n2g"
                vstt(Amod[:, b, l * 32 + which * 16:l * 32 + which * 16 + 16],
                     modv[:, b, l * 96 + which * 48 + 16:l * 96 + which * 48 + 32], 1.0,
                     ppt[:, PO[gname] + l * 16:PO[gname] + (l + 1) * 16], ALU.add, ALU.mult,
                     [("modv", l, which * 48 + 16 + i) for i in range(16)] + ["ppt"], [("Amod", b, l, which)])
        yield

    for _ in mod_gen(0):
        pass
    bg_gen = [None]

    def modcol(b, l, v, j):
        o = l * 96 + v * 16 + j
        return modv[:, b, o:o + 1]

    def hkeys(par):
        return [("hT", par, kc) for kc in range(KC)]

    def norm_to_hT(b, l, which, par):
        xs, hT = xs2[par], hT2[par]
        for kc in range(KC):
            if kc % 2 == 0:
                act(hT[:, kc, :], xs[:, kc, :], AF.Square, [("xs", par, kc)], [("hT", par, kc)])
            else:
                vtt(hT[:, kc, :], xs[:, kc, :], xs[:, kc, :], ALU.mult, [("xs", par, kc)], [("hT", par, kc)])
        bk = nbank()
        mmg(bk, [(ones_b[:], hT[:, kc, :]) for kc in range(KC)], ["ones_b"] + hkeys(par))
        act(tm["t_s1"][:], ps[:, bk, :], AF.Sqrt, [("ps", bk), "cst0"], ["t_s1"], bias=cst[:, 0:1], scale=1.0 / D)
        R.op("vector", lambda e: e.reciprocal(out=tm["rstd"][:], in_=tm["t_s1"][:]), reads=["t_s1"], writes=["rstd"])
        if which == 2:
            return
        aoff = l * 32 + which * 16
        for kc in range(KC):
            tn = "t_v1" if kc % 2 == 0 else "t_v2"
            vtt(tm[tn][:], xs[:, kc, :], tm["rstd"][:], ALU.mult, [("xs", par, kc), "rstd"], [tn])
            act(hT[:, kc, :], tm[tn][:], AF.Identity, [tn, ("Amod", b, l, which), ("modv", l, which * 48 + kc)],
                [("hT", par, kc)], bias=modcol(b, l, which * 3, kc), scale=Amod[:, b, aoff + kc:aoff + kc + 1])

    def win_tiles(l, c0, nw):
        if nw <= 256:
            w, k = wtile(win_d[l * D:(l + 1) * D, c0:c0 + nw], 16, nw)
            return (lambda kc, a, bb: w[:, kc, a:bb]), [k]
        w0, k0 = wtile(win_d[l * D:l * D + 1024, c0:c0 + nw], 8, nw)
        w1, k1 = wtile(win_d[l * D + 1024:(l + 1) * D, c0:c0 + nw], 8, nw)
        return (lambda kc, a, bb: (w0 if kc < 8 else w1)[:, kc % 8, a:bb]), [k0, k1]

    def mixer(b, l, ti, par, do_norm=True, mid_hook=None):
        xs, hT = xs2[par], hT2[par]
        hT_all = hkeys(par)
        if do_norm:
            norm_to_hT(b, l, 0, par)
        R.dma("gpsimd", lambda e: e.dma_start(out=pwt[:], in_=pw_d[l * 128:(l + 1) * 128, :]), "pw", writes=["pwt"])

        def ZA(c):
            gx, kx = win_tiles(l, c * 128, 128)
            gg, kg = win_tiles(l, DR + c * 128, 128)
            b1 = nbank()
            mmg(b1, [(gx(kc, 0, 128), hT[:, kc, :]) for kc in range(KC)], kx + hT_all)
            b2 = nbank()
            mmg(b2, [(gg(kc, 0, 128), hT[:, kc, :]) for kc in range(KC)], kg + hT_all)
            return b1, b2

        nxt = ZA(0)
        for c in range(NCH):
            bax, bag = nxt
            if c + 1 < NCH:
                nxt = ZA(c + 1)
            lc = l * NCH + c
            ab = axbuf[c % 2]
            abk = "axbuf%d" % (c % 2)
            if ti == 0:
                R.op("vector", lambda e, ab=ab: e.memset(ab[:, 0:3], 0.0), writes=[abk + "h"])
            else:
                R.op("vector", lambda e, ab=ab, lc=lc: e.tensor_copy(out=ab[:, 0:3], in_=sA[:, lc, :]),
                     reads=[("sA", lc)], writes=[abk + "h"])
            act(ab[:, 3:T + 3], ps[:, bax, :], AF.Copy, [("ps", bax)], [abk])
            R.op("vector", lambda e, ab=ab, lc=lc: e.tensor_copy(out=sA[:, lc, :], in_=ab[:, T:T + 3]),
                 reads=[abk], writes=[("sA", lc)])
            xc = tm["t_xc"]
            vts(xc[:], ab[:, 0:T], col("caw", lc * 4 + 0), col("cab", lc), ALU.mult, ALU.add, [abk, abk + "h", "ppt"], ["t_xc"])
            for j in range(1, 4):
                vstt(xc[:], ab[:, j:j + T], col("caw", lc * 4 + j), xc[:], ALU.mult, ALU.add,
                     [abk, abk + "h", "t_xc", "ppt"], ["t_xc"])
            act(xcb[:], xc[:], AF.Copy, ["t_xc"], ["xcb"])
            br = nbank()
            mmg(br, [(pwt[:, c * 128:(c + 1) * 128], xcb[:])], ["pwt", "xcb"])
            bi = nbank()
            mmg(bi, [(pwt[:, (NCH + c) * 128:(NCH + c + 1) * 128], xcb[:])], ["pwt", "xcb"])
            act(tm["t_r"][:], ps[:, br, :], AF.Sigmoid, [("ps", br), "ppt"], ["t_r"], bias=col("bra", lc))
            act(tm["t_i"][:], ps[:, bi, :], AF.Sigmoid, [("ps", bi), "ppt"], ["t_i"], bias=col("brx", lc))
            act(tm["t_a"][:], tm["t_r"][:], AF.Exp, ["t_r", "c1"], ["t_a"], scale=c1t[:, lc:lc + 1])
            act(tm["t_m"][:], tm["t_a"][:], AF.Square, ["t_a"], ["t_m"])
            act(tm["t_m"][:], tm["t_m"][:], AF.Sqrt, ["t_m", "cst"], ["t_m"], bias=cst[:, 1:2], scale=-1.0)
            vtt(tm["t_u"][:], tm["t_i"][:], xc[:], ALU.mult, ["t_i", "t_xc"], ["t_u"])
            vtt(tm["t_u"][:], tm["t_u"][:], tm["t_m"][:], ALU.mult, ["t_u", "t_m"], ["t_u"])
            if ti == 0:
                R.op("vector", lambda e: e.tensor_tensor_scan(out=tm["t_h"][:], data0=tm["t_a"][:], data1=tm["t_u"][:],
                                                              initial=0.0, op0=ALU.mult, op1=ALU.add),
                     reads=["t_a", "t_u"], writes=["t_h"])
            else:
                R.op("vector", lambda e, lc=lc: e.tensor_tensor_scan(out=tm["t_h"][:], data0=tm["t_a"][:], data1=tm["t_u"][:],
                                                                     initial=sH[:, lc:lc + 1], op0=ALU.mult, op1=ALU.add),
                     reads=["t_a", "t_u", ("sH", lc)], writes=["t_h"])
            R.op("vector", lambda e, lc=lc: e.tensor_copy(out=sH[:, lc:lc + 1], in_=tm["t_h"][:, T - 1:T]),
                 reads=["t_h"], writes=[("sH", lc)])
            act(tm["t_gl"][:], ps[:, bag, :], AF.Gelu_apprx_tanh, [("ps", bag)], ["t_gl"])
            vtt(pAB[:, c, :], tm["t_gl"][:], tm["t_h"][:], ALU.mult, ["t_gl", "t_h"], [("pab", c)])
        if mid_hook is not None:
            mid_hook()

        def ZB(c):
            out = []
            for r in range(3):
                g_, k_ = win_tiles(l, 2 * DR + r * DR + c * 128, 128)
                bk = nbank()
                mmg(bk, [(g_(kc, 0, 128), hT[:, kc, :]) for kc in range(KC)], k_ + hT_all)
                out.append(bk)
            return out

        nxt = ZB(0)
        for c in range(NCH):
            bv, bbg, bcg = nxt
            if c + 1 < NCH:
                nxt = ZB(c + 1)
            lc = l * NCH + c
            if ti == 0:
                R.op("vector", lambda e: e.memset(cvbuf[:, 0:2], 0.0), writes=["cvh"])
            else:
                R.op("vector", lambda e, lc=lc: e.tensor_copy(out=cvbuf[:, 0:2], in_=sBt[:, lc, :]),
                     reads=[("sB", lc)], writes=["cvh"])
            act(tm["t_s1"][:], ps[:, bcg, :], AF.Copy, [("ps", bcg)], ["t_s1"])
            vtt(cvbuf[:, 2:T + 2], tm["t_s1"][:], ps[:, bv, :], ALU.mult, ["t_s1", ("ps", bv)], ["cv"])
            R.op("vector", lambda e, lc=lc: e.tensor_copy(out=sBt[:, lc, :], in_=cvbuf[:, T:T + 2]),
                 reads=["cv"], writes=[("sB", lc)])
            cvo = tm["t_v1"]
            vts(cvo[:], cvbuf[:, 0:T], col("cbw", lc * 3 + 0), col("cbb", lc), ALU.mult, ALU.add, ["cv", "cvh", "ppt"], ["t_v1"])
            for j in range(1, 3):
                vstt(cvo[:], cvbuf[:, j:j + T], col("cbw", lc * 3 + j), cvo[:], ALU.mult, ALU.add,
                     ["cv", "cvh", "t_v1", "ppt"], ["t_v1"])
            vtt(pAB[:, 8 + c, :], cvo[:], ps[:, bbg, :], ALU.mult, ["t_v1", ("ps", bbg)], [("pab", 8 + c)])

        pA_all = [("pab", c) for c in range(NCH)]
        pB_all = [("pab", 8 + c) for c in range(NCH)]
        for jp in range(8):
            cs = slice(jp * 256, (jp + 1) * 256)
            j0, j1 = jp * 2, jp * 2 + 1
            sl0, sl1 = slice(0, 128), slice(128, 256)
            woa, ka = wtile(woa_d[l * DR:(l + 1) * DR, cs], 8, 256)
            bya0 = nbank()
            mmg(bya0, [(woa[:, c, sl0], pAB[:, c, :]) for c in range(NCH)], [ka] + pA_all)
            bya1 = nbank()
            mmg(bya1, [(woa[:, c, sl1], pAB[:, c, :]) for c in range(NCH)], [ka] + pA_all)
            gga, kga = win_tiles(l, 5 * DR + jp * 256, 256)
            bga0 = nbank()
            mmg(bga0, [(gga(kc, 0, 128), hT[:, kc, :]) for kc in range(KC)], kga + hT_all)
            bga1 = nbank()
            mmg(bga1, [(gga(kc, 128, 256), hT[:, kc, :]) for kc in range(KC)], kga + hT_all)
            act(tm["t_r"][:], ps[:, bga0, :], AF.Sigmoid, [("ps", bga0)], ["t_r"])
            vtt(tm["t_a"][:], tm["t_r"][:], ps[:, bya0, :], ALU.mult, ["t_r", ("ps", bya0)], ["t_a"])
            act(tm["t_i"][:], ps[:, bga1, :], AF.Sigmoid, [("ps", bga1)], ["t_i"])
            vtt(tm["t_u"][:], tm["t_i"][:], ps[:, bya1, :], ALU.mult, ["t_i", ("ps", bya1)], ["t_u"])
            wob, kb = wtile(wob_d[l * DR:(l + 1) * DR, cs], 8, 256)
            byb0 = nbank()
            mmg(byb0, [(wob[:, c, sl0], pAB[:, 8 + c, :]) for c in range(NCH)], [kb] + pB_all)
            byb1 = nbank()
            mmg(byb1, [(wob[:, c, sl1], pAB[:, 8 + c, :]) for c in range(NCH)], [kb] + pB_all)
            ggb, kgb = win_tiles(l, 5 * DR + D + jp * 256, 256)
            bgb0 = nbank()
            mmg(bgb0, [(ggb(kc, 0, 128), hT[:, kc, :]) for kc in range(KC)], kgb + hT_all)
            bgb1 = nbank()
            mmg(bgb1, [(ggb(kc, 128, 256), hT[:, kc, :]) for kc in range(KC)], kgb + hT_all)
            act(tm["t_xc"][:], ps[:, bgb0, :], AF.Sigmoid, [("ps", bgb0)], ["t_xc"])
            vtt(tm["t_h"][:], tm["t_xc"][:], ps[:, byb0, :], ALU.mult, ["t_xc", ("ps", byb0)], ["t_h"])
            vtt(mg[:, j0, :], tm["t_a"][:], tm["t_h"][:], ALU.add, ["t_a", "t_h"], [("mg", j0)])
            act(tm["t_gl"][:], ps[:, bgb1, :], AF.Sigmoid, [("ps", bgb1)], ["t_gl"])
            vtt(tm["t_v1"][:], tm["t_gl"][:], ps[:, byb1, :], ALU.mult, ["t_gl", ("ps", byb1)], ["t_v1"])
            vtt(mg[:, j1, :], tm["t_u"][:], tm["t_v1"][:], ALU.add, ["t_u", "t_v1"], [("mg", j1)])
        mg_all = [("mg", j) for j in range(KC)]
        for jg in range(4):
            w0, k0 = wtile(wo_d[l * D:l * D + 1024, jg * 512:(jg + 1) * 512], 8, 512)
            w1, k1 = wtile(wo_d[l * D + 1024:(l + 1) * D, jg * 512:(jg + 1) * 512], 8, 512)
            for j4 in range(4):
                j = jg * 4 + j4
                sl = slice(j4 * 128, (j4 + 1) * 128)
                bo = nbank()
                mmg(bo, [((w0 if k < 8 else w1)[:, k % 8, sl], mg[:, k, :]) for k in range(KC)], [k0, k1] + mg_all)
                vstt(xs[:, j, :], ps[:, bo, :], modcol(b, l, 2, j), xs[:, j, :], ALU.mult, ALU.add,
                     [("ps", bo), ("modv", l, 32 + j), ("xs", par, j)], [("xs", par, j)])

    def emit_gbc(e_):
        buf = e_ % 2
        for par in range(2):
            bk = nbank()
            for s in range(4):
                d_ = dg[s % 2]
                dk = "dg%d" % (s % 2)
                vts(d_[:], ident_f[:], gate[:, par, s, e_:e_ + 1], None, ALU.mult, None, ["ident", ("gate", par, s)], [dk])
                R.op("tensor", lambda e, d_=d_, s=s, bk=bk: e.matmul(ps[:, bk, s * 128:(s + 1) * 128], lhsT=ones_f[:], rhs=d_[:],
                                                                      start=True, stop=True),
                     reads=[dk, "ones_f"], writes=[("ps", bk)] if s == 0 else [("psx", bk, s)])
            act(gbcj[:, buf, par, :], ps[:, bk, :], AF.Copy, [("ps", bk)] + [("psx", bk, s) for s in range(1, 4)],
                [("gbc", buf, par)])

    def ffn_groups(b, l, groups):
        seen = set()

        def GU(gi):
            gsrc, usrc, dsrc, e_ = groups[gi]
            if e_ is not None and e_ not in seen:
                seen.add(e_)
                emit_gbc(e_)
            for half in range(2):
                gt, kg = wtile(gsrc(half), 16, 256)
                ut, ku = wtile(usrc(half), 16, 256)
                for q2 in range(2):
                    q = half * 2 + q2
                    sl = slice(q2 * 128, (q2 + 1) * 128)
                    for par in range(2):
                        hT = hT2[par]
                        bg = nbank()
                        mmg(bg, [(gt[:, kc, sl], hT[:, kc, :]) for kc in range(KC)], [kg] + hkeys(par))
                        bu = nbank()
                        mmg(bu, [(ut[:, kc, sl], hT[:, kc, :]) for kc in range(KC)], [ku] + hkeys(par))
                        ch = (gi % 2) * 8 + par * 4 + q
                        act(tm["t_s2"][:], ps[:, bg, :], AF.Silu, [("ps", bg)], ["t_s2"])
                        if e_ is not None:
                            vtt(tm["t_s2"][:], tm["t_s2"][:], gbcj[:, e_ % 2, par, :], ALU.mult,
                                ["t_s2", ("gbc", e_ % 2, par)], ["t_s2"])
                        vtt(pAB[:, ch, :], tm["t_s2"][:], ps[:, bu, :], ALU.mult, ["t_s2", ("ps", bu)], [("pab", ch)])

        def DN(gi):
            gsrc, usrc, dsrc, e_ = groups[gi]
            d0, kd0 = wtile(dsrc(0), 4, 1024)
            d1, kd1 = wtile(dsrc(1), 4, 1024)
            for j in range(KC):
                dd, kd = (d0, kd0) if j < 8 else (d1, kd1)
                sl = slice((j % 8) * 128, (j % 8 + 1) * 128)
                for par in range(2):
                    base = (gi % 2) * 8 + par * 4
                    bd = nbank()
                    mmg(bd, [(dd[:, k, sl], pAB[:, base + k, :]) for k in range(4)],
                        [kd] + [("pab", base + k) for k in range(4)])
                    vstt(xs2[par][:, j, :], ps[:, bd, :], modcol(b, l, 5, j), xs2[par][:, j, :], ALU.mult, ALU.add,
                         [("ps", bd), ("modv", l, 80 + j), ("xs", par, j)], [("xs", par, j)])

        GU(0)
        per = -(-26 // len(groups))
        for gi in range(len(groups)):
            if gi + 1 < len(groups):
                GU(gi + 1)
            DN(gi)
            if bg_gen[0] is not None:
                for _ in range(per):
                    if next(bg_gen[0], "end") == "end":
                        bg_gen[0] = None
                        break
        if bg_gen[0] is not None:
            for _ in bg_gen[0]:
                pass
            bg_gen[0] = None

    def ffn_dense(b, l):
        ld = l // 2
        norm_to_hT(b, l, 1, 1)
        groups = []
        for g in range(DFF // 512):
            groups.append((
                lambda half, g=g: wfg_d[ld * D:(ld + 1) * D, g * 512 + half * 256:g * 512 + (half + 1) * 256],
                lambda half, g=g: wfu_d[ld * D:(ld + 1) * D, g * 512 + half * 256:g * 512 + (half + 1) * 256],
                lambda hh, g=g: wfd_d[ld * DFF + g * 512:ld * DFF + (g + 1) * 512, hh * 1024:(hh + 1) * 1024],
                None))
        ffn_groups(b, l, groups)

    def ffn_moe(b, l):
        lm = l // 2
        ro = 2 * NCH * 128
        for par in range(2):
            if par == 1:
                norm_to_hT(b, l, 1, par)
            hT = hT2[par]
            for s in range(4):
                bk = nbank()
                mmg(bk, [(hT[:, kc, s * 128:(s + 1) * 128], pwt[:, ro + kc * 8:ro + (kc + 1) * 8]) for kc in range(KC)],
                    ["pwt"] + hkeys(par), ncols=8)
                gk = ("gate", par, s)
                lk = ("lg", par, s)
                vtt(lg[:, par, s, :], ps[:, bk, 0:8], ppt[:, PO["brt"] + lm * 8:PO["brt"] + (lm + 1) * 8], ALU.add,
                    [("ps", bk), "ppt"], [lk])
                R.op("vector", lambda e, s=s, par=par: e.max(out=m8[:], in_=lg[:, par, s, :]), reads=[lk], writes=["m8"])
                vts(sm[:], lg[:, par, s, :], m8[:, 1:2], None, ALU.is_ge, None, [lk, "m8"], ["sm"])
                vts(sc4[:, 0:1], m8[:, 0:1], -1.0, None, ALU.mult, None, ["m8"], ["m8n"])
                act(gate[:, par, s, :], lg[:, par, s, :], AF.Exp, [lk, "m8n"], [gk], bias=sc4[:, 0:1])
                vtt(gate[:, par, s, :], gate[:, par, s, :], sm[:], ALU.mult, [gk, "sm"], [gk])
                R.op("vector", lambda e, s=s, par=par: e.tensor_reduce(out=sc4[:, 1:2], in_=gate[:, par, s, :], axis=AX.X,
                                                                       op=ALU.add), reads=[gk], writes=["den"])
                R.op("vector", lambda e: e.reciprocal(out=sc4[:, 2:3], in_=sc4[:, 1:2]), reads=["den"], writes=["rden"])
                vts(gate[:, par, s, :], gate[:, par, s, :], sc4[:, 2:3], None, ALU.mult, None, [gk, "rden"], [gk])
        groups = []
        for e_ in range(NE):
            base = (lm * NE + e_) * D
            dbase = (lm * NE + e_) * DFFE
            for g in range(DFFE // 512):
                groups.append((
                    lambda half, g=g, base=base: weg_d[base:base + D, g * 512 + half * 256:g * 512 + (half + 1) * 256],
                    lambda half, g=g, base=base: weu_d[base:base + D, g * 512 + half * 256:g * 512 + (half + 1) * 256],
                    lambda hh, g=g, dbase=dbase: wed_d[dbase + g * 512:dbase + (g + 1) * 512, hh * 1024:(hh + 1) * 1024],
                    e_))
        ffn_groups(b, l, groups)

    n = 0
    for b in range(NB):
        for pair in range(NTILE // 2):
            for par in range(2):
                ti = pair * 2 + par
                r0 = b * S + ti * T
                xs = xs2[par]
                for s in range(4):
                    for q in range(4):
                        xt = xtok[n % 2]
                        xk = "xtok%d" % (n % 2)
                        R.dma("sync", lambda e, xt=xt, s=s, q=q, r0=r0: e.dma_start(
                            out=xt[:], in_=x_d[r0 + s * 128:r0 + (s + 1) * 128, q * 512:(q + 1) * 512]), xk, writes=[xk])
                        bk = nbank()

                        def tp(e, xt=xt, bk=bk):
                            ins = None
                            for i in range(4):
                                ins = e.transpose(out=ps[:, bk, i * 128:(i + 1) * 128], in_=xt[:, i * 128:(i + 1) * 128],
                                                  identity=ident_f[:])
                            return ins
                        R.op("tensor", tp, reads=[xk, "ident"], writes=[("ps", bk)])
                        src3 = ps[:, bk, :].rearrange("p (i c) -> p i c", c=128)
                        dst3 = xs[:, q * 4:(q + 1) * 4, s * 128:(s + 1) * 128]
                        wk = [("xs", par, q * 4 + i) for i in range(4)]
                        if n % 2 == 0:
                            act(dst3, src3, AF.Copy, [("ps", bk)], wk)
                        else:
                            R.op("vector", lambda e, dst3=dst3, src3=src3: e.tensor_copy(out=dst3, in_=src3),
                                 reads=[("ps", bk)], writes=wk)
                        n += 1
            for l in range(L):
                if "mixer" not in SKIP:
                    mixer(b, l, pair * 2, 0, mid_hook=lambda b=b, l=l: norm_to_hT(b, l, 0, 1))
                if "ffn" not in SKIP:
                    norm_to_hT(b, l, 1, 0)
                if "mixer" not in SKIP:
                    mixer(b, l, pair * 2 + 1, 1, do_norm=False)
                if "ffn" in SKIP:
                    continue
                if b == 0 and pair == 0 and l + 1 < L:
                    bg_gen[0] = mod_gen(l + 1)
                if l % 2 == 0:
                    ffn_dense(b, l)
                else:
                    ffn_moe(b, l)
            for par in range(2):
                ti = pair * 2 + par
                r0 = b * S + ti * T
                xs = xs2[par]
                norm_to_hT(b, 0, 2, par)
                for kc in range(KC):
                    vstt(xs[:, kc, :], xs[:, kc, :], col("fg", kc), tm["rstd"][:], ALU.mult, ALU.mult,
                         [("xs", par, kc), "rstd", "ppt"], [("xs", par, kc)])
                for s in range(4):
                    for q in range(4):
                        bk = nbank()

                        def tp2(e, s=s, q=q, bk=bk, xs=xs):
                            ins = None
                            for i in range(4):
                                ins = e.transpose(out=ps[:, bk, i * 128:(i + 1) * 128],
                                                  in_=xs[:, q * 4 + i, s * 128:(s + 1) * 128], identity=ident_f[:])
                            return ins
                        R.op("tensor", tp2, reads=[("xs", par, q * 4 + i) for i in range(4)] + ["ident"], writes=[("ps", bk)])
                        xt = xtok[n % 2]
                        xk = "xtok%d" % (n % 2)
                        n += 1
                        act(xt[:], ps[:, bk, :], AF.Copy, [("ps", bk)], [xk])
                        R.dma("sync", lambda e, xt=xt, s=s, q=q, r0=r0: e.dma_start(
                            out=y_d[r0 + s * 128:r0 + (s + 1) * 128, q * 512:(q + 1) * 512], in_=xt[:]), "o" + xk,
                            reads=[xk], writes=["out_" + xk])
    R.wait_all("sync", ["out_xtok0", "out_xtok1"])
    stats = R.emit(nc, st)
    st.close()
    return nc, stats


def host_pack(inp, cfg, core, ncores):
    NB, S, L = cfg["NB"], cfg["S"], cfg["L"]
    NE = cfg["NE"]
    LM = L // 2
    PO, NPP = pp_layout(NB, L, LM)
    pp = np.zeros((128, NPP), np.float32)
    f = lambda a: np.asarray(a, np.float32)
    c = f(inp["c"])[core * NB:(core + 1) * NB]
    pp[:, PO["c"]:PO["c"] + NB * 16] = c.reshape(NB, 16, 128).transpose(2, 0, 1).reshape(128, NB * 16)
    pp[:, PO["bmod"]:PO["bmod"] + L * 96] = f(inp["b_mod"])[:L].reshape(L, 96, 128).transpose(2, 0, 1).reshape(128, L * 96)
    pp[:, PO["n1g"]:PO["n1g"] + L * 16] = f(inp["norm1_g"])[:L].reshape(L, 16, 128).transpose(2, 0, 1).reshape(128, L * 16)
    pp[:, PO["n2g"]:PO["n2g"] + L * 16] = f(inp["norm2_g"])[:L].reshape(L, 16, 128).transpose(2, 0, 1).reshape(128, L * 16)
    pp[:, PO["fg"]:PO["fg"] + 16] = f(inp["final_g"]).reshape(16, 128).T
    pp[:, PO["caw"]:PO["caw"] + L * 32] = f(inp["conv_a_w"])[:L].reshape(L, 4, 8, 128).transpose(3, 0, 2, 1).reshape(128, L * 32)
    for name, key in (("cab", "conv_a_b"), ("bra", "b_rg_a"), ("brx", "b_rg_x"), ("lam", "rg_lambda"), ("cbb", "conv_b_b")):
        pp[:, PO[name]:PO[name] + L * 8] = f(inp[key])[:L].reshape(L, 8, 128).transpose(2, 0, 1).reshape(128, L * 8)
    pp[:, PO["cbw"]:PO["cbw"] + L * 24] = f(inp["conv_b_w"])[:L].reshape(L, 3, 8, 128).transpose(3, 0, 2, 1).reshape(128, L * 24)
    if LM > 0:
        pp[:, PO["brt"]:PO["brt"] + LM * 8] = np.broadcast_to(f(inp["b_router"])[:LM].reshape(1, LM * 8), (128, LM * 8))
    NPW = 2 * NCH * 128 + KC * 8
    pw = np.zeros((L, 128, NPW), np.float32)
    for gi, key in enumerate(("w_rg_a", "w_rg_x")):
        w = f(inp[key])[:L]
        for c_ in range(NCH):
            for hh in range(2):
                pw[:, hh * 64:(hh + 1) * 64, (gi * NCH + c_) * 128 + hh * 64:(gi * NCH + c_) * 128 + (hh + 1) * 64] = w[:, 2 * c_ + hh]
    for l in range(L):
        if l % 2 == 1:
            wr = f(inp["w_router"])[l // 2]
            pw[l, :, 2 * NCH * 128:] = wr.reshape(16, 128, NE).transpose(1, 0, 2).reshape(128, 16 * NE)
    return pp, pw.reshape(L * 128, NPW)


FULL = dict(NB=1, S=2048, L=4, DFF=6144, DFFE=3072, NE=8)
NCORES = 8
_cache = {}


def run(inp, cfg, ncores):
    NB, S, L = cfg["NB"], cfg["S"], cfg["L"]
    LD, LM = (L + 1) // 2, L // 2
    key = tuple(sorted((k, v) for k, v in cfg.items()))
    if key not in _cache:
        _cache[key] = build(cfg)
    nc, stats = _cache[key]
    f = lambda a: np.ascontiguousarray(np.asarray(a, np.float32))
    shared = {
        "ident": np.eye(128, dtype=np.float32),
        "w_mod": f(inp["w_mod"])[:L].reshape(L * D, 6 * D),
        "w_in": f(inp["w_in"])[:L].reshape(L * D, DIN),
        "w_out_a": f(inp["w_out_a"])[:L].reshape(L * DR, D),
        "w_out_b": f(inp["w_out_b"])[:L].reshape(L * DR, D),
        "w_o": f(inp["w_o"])[:L].reshape(L * D, D),
        "w_ff_gate": f(inp["w_ff_gate"])[:LD].reshape(LD * D, cfg["DFF"]),
        "w_ff_up": f(inp["w_ff_up"])[:LD].reshape(LD * D, cfg["DFF"]),
        "w_ff_down": f(inp["w_ff_down"])[:LD].reshape(LD * cfg["DFF"], D),
        "w_e_gate": f(inp["w_e_gate"])[:max(LM, 1)].reshape(-1, cfg["DFFE"]),
        "w_e_up": f(inp["w_e_up"])[:max(LM, 1)].reshape(-1, cfg["DFFE"]),
        "w_e_down": f(inp["w_e_down"])[:max(LM, 1)].reshape(-1, D),
    }
    x = f(inp["x"])
    in_maps = []
    for core in range(ncores):
        pp, pw = host_pack(inp, cfg, core, ncores)
        m = dict(shared)
        m["x"] = x[core * NB:(core + 1) * NB].reshape(NB * S, D)
        m["pp"] = pp
        m["pw"] = pw
        in_maps.append(m)
    res = run_bass_kernel_spmd(nc, in_maps, core_ids=list(range(ncores)))
    out = np.concatenate([r["y"].reshape(NB, S, D) for r in res.results], axis=0)
    return out.astype(np.float32)


def kernel(**inputs):
    return run(inputs, FULL, NCORES)
```

```python
from contextlib import ExitStack
import numpy as np
import concourse.bass as bass
import concourse.mybir as mybir
from concourse.bass_utils import run_bass_kernel_spmd

F32 = mybir.dt.float32
BF16 = mybir.dt.bfloat16
AF = mybir.ActivationFunctionType
ALU = mybir.AluOpType
AX = mybir.AxisListType

ENGS = ("tensor", "vector", "scalar", "gpsimd", "sync")
D = 2048
KC = 16
T = 512
DR = 1024
NCH = 8
EPS = 1e-6
EPOCH = 16000
DIN = 9216


class Ev:
    __slots__ = ("eng", "sem", "val", "signaled", "clock")

    def __init__(self, eng):
        self.eng = eng
        self.sem = None
        self.val = None
        self.signaled = False
        self.clock = None


class Rec:
    def __init__(self):
        self.q = {e: [] for e in ENGS}
        self.last_w = {}
        self.readers = {}
        self.dma_cnt = {}
        self.dma_ep = {}
        self.alias = {}
        self.order = []

    def _deps(self, reads, writes):
        deps = []
        for k in reads:
            w = self.last_w.get(k)
            if w is not None:
                deps.append(w)
        for k in writes:
            w = self.last_w.get(k)
            if w is not None:
                deps.append(w)
            deps.extend(self.readers.get(k, ()))
        return deps

    def _commit(self, ev, reads, writes):
        for k in reads:
            self.readers.setdefault(k, []).append(ev)
        for k in writes:
            self.last_w[k] = ev
            self.readers[k] = []

    def _canon(self, keys):
        a = self.alias
        return [a.get(k, k) for k in keys]

    def op(self, eng, fn, reads=(), writes=()):
        reads = self._canon(reads)
        writes = self._canon(writes)
        deps = self._deps(reads, writes)
        ev = Ev(eng)
        for d in deps:
            d.signaled = True
        self.order.append((eng, len(self.q[eng])))
        self.q[eng].append(("op", fn, ev, deps))
        self._commit(ev, reads, writes)
        return ev

    def dma(self, eng, fn, semname, reads=(), writes=()):
        reads = self._canon(reads)
        writes = self._canon(writes)
        deps = self._deps(reads, writes)
        ev = Ev(eng)
        ev.signaled = True
        ep = self.dma_ep.get(semname, 0)
        if self.dma_cnt.get((semname, ep), 0) + 16 > EPOCH:
            ep += 1
            self.dma_ep[semname] = ep
        self.dma_cnt[(semname, ep)] = self.dma_cnt.get((semname, ep), 0) + 16
        ev.sem = (semname, ep)
        ev.val = self.dma_cnt[(semname, ep)]
        for d in deps:
            d.signaled = True
        self.order.append((eng, len(self.q[eng])))
        self.q[eng].append(("dma", fn, ev, deps))
        self._commit(ev, reads, writes)
        return ev

    def wait_all(self, eng, keys):
        keys = self._canon(keys)
        deps = [self.last_w[k] for k in keys if k in self.last_w]
        for d in deps:
            d.signaled = True
        self.order.append((eng, len(self.q[eng])))
        self.q[eng].append(("wait", None, None, deps))

    def emit(self, nc, stack):
        prog = {}
        dsem = {n: stack.enter_context(nc.semaphore("dma_%s_%d" % n)) for n in self.dma_cnt}
        for e in ENGS:
            n = 0
            for kind, fn, ev, deps in self.q[e]:
                if kind == "op" and ev.signaled:
                    k = n // EPOCH
                    n += 1
                    if (e, k) not in prog:
                        prog[(e, k)] = stack.enter_context(nc.semaphore("prog_%s_%d" % (e, k)))
                    ev.sem = prog[(e, k)]
                    ev.val = n - k * EPOCH
                elif kind == "dma":
                    ev.sem = dsem[ev.sem]
        clock = {e: {} for e in ENGS}
        waits = {e: [None] * len(self.q[e]) for e in ENGS}
        for e, idx in self.order:
            kind, fn, ev, deps = self.q[e][idx]
            ck = clock[e]
            need = {}
            for d in deps:
                key = id(d.sem)
                if ck.get(key, 0) < d.val and need.get(key, (None, 0))[1] < d.val:
                    need[key] = (d.sem, d.val)
            for d in deps:
                for k2, v2 in d.clock.items():
                    if ck.get(k2, 0) < v2:
                        ck[k2] = v2
                key = id(d.sem)
                if ck.get(key, 0) < d.val:
                    ck[key] = d.val
            waits[e][idx] = list(need.values())
            if ev is not None and (kind == "dma" or ev.signaled):
                c2 = dict(ck)
                c2[id(ev.sem)] = max(c2.get(id(ev.sem), 0), ev.val)
                ev.clock = c2
        block = stack.enter_context(nc.Block())
        stats = {}

        def run(e):
            def body(engine):
                nw = 0
                for idx, (kind, fn, ev, deps) in enumerate(self.q[e]):
                    for sem, val in waits[e][idx]:
                        engine.wait_ge(sem, val)
                        nw += 1
                    if kind == "wait":
                        continue
                    ins = fn(engine)
                    if kind == "dma":
                        ins.then_inc(ev.sem, 16)
                    elif ev.signaled:
                        ins.then_inc(ev.sem, 1)
                stats[e] = (len(self.q[e]), nw)
            return body

        block.tensor(run("tensor"))
        block.vector(run("vector"))
        block.scalar(run("scalar"))
        block.gpsimd(run("gpsimd"))
        block.sync(run("sync"))
        return stats


def pp_layout(NB, L, LM):
    off = {}
    n = 0
    for name, w in (("c", NB * 16), ("bmod", L * 96), ("n1g", L * 16), ("n2g", L * 16), ("fg", 16),
                    ("caw", L * 32), ("cab", L * 8), ("bra", L * 8), ("brx", L * 8), ("lam", L * 8),
                    ("cbw", L * 24), ("cbb", L * 8), ("brt", max(LM, 1) * 8)):
        off[name] = n
        n += w
    return off, n


def build(cfg):
    NB, S, L = cfg["NB"], cfg["S"], cfg["L"]
    DFF, DFFE, NE = cfg["DFF"], cfg["DFFE"], cfg["NE"]
    LD, LM = (L + 1) // 2, L // 2
    NTILE = S // T
    assert NTILE % 2 == 0
    NSLOT = 4
    SLOTE = 4096
    PO, NPP = pp_layout(NB, L, LM)
    NPW = 2 * NCH * 128 + KC * 8
    SKIP = cfg.get("skip", ())

    nc = bass.Bass("TRN2", target_bir_lowering=False)
    dt = lambda name, shape, kind="ExternalInput": nc.dram_tensor(name, shape, F32, kind=kind).ap()
    x_d = dt("x", [NB * S, D])
    pp_d = dt("pp", [128, NPP])
    pw_d = dt("pw", [L * 128, NPW])
    id_d = dt("ident", [128, 128])
    wmod_d = dt("w_mod", [L * D, 6 * D])
    win_d = dt("w_in", [L * D, DIN])
    woa_d = dt("w_out_a", [L * DR, D])
    wob_d = dt("w_out_b", [L * DR, D])
    wo_d = dt("w_o", [L * D, D])
    wfg_d = dt("w_ff_gate", [LD * D, DFF])
    wfu_d = dt("w_ff_up", [LD * D, DFF])
    wfd_d = dt("w_ff_down", [LD * DFF, D])
    weg_d = dt("w_e_gate", [max(LM, 1) * NE * D, DFFE])
    weu_d = dt("w_e_up", [max(LM, 1) * NE * D, DFFE])
    wed_d = dt("w_e_down", [max(LM, 1) * NE * DFFE, D])
    y_d = dt("y", [NB * S, D], kind="ExternalOutput")

    R = Rec()
    R.alias = {"t_s2": "t_gl", "t_v2": "t_m", "t_s1": "t_u", "xtok0": "t_r", "xtok1": "t_i"}
    st = ExitStack()
    sb = lambda name, shape, dtype=F32: st.enter_context(nc.sbuf_tensor(name, shape, dtype))
    xs2 = [sb("xs%d" % i, [128, KC, T]) for i in range(2)]
    hT2 = [sb("hT%d" % i, [128, KC, T], BF16) for i in range(2)]
    pAB = sb("pAB", [128, 16, T], BF16)
    mg = sb("mg", [128, KC, T], BF16)
    wsl = [sb("wsl%d" % i, [128, SLOTE], BF16) for i in range(NSLOT)]
    gbcj = sb("gbcj", [128, 2, 2, T], BF16)
    ppt = sb("ppt", [128, NPP])
    pwt = sb("pwt", [128, NPW], BF16)
    modv = sb("modv", [128, NB, L * 96])
    Amod = sb("Amod", [128, NB, L * 32])
    c1t = sb("c1t", [128, L * 8])
    cact = sb("cact", [128, KC, NB], BF16)
    ident_f = sb("ident_f", [128, 128])
    ones_f = sb("ones_f", [128, 128])
    ones_b = sb("ones_b", [128, 128], BF16)
    cst = sb("cst", [128, 2])
    sA = sb("sA", [128, L * NCH, 3])
    sBt = sb("sBt", [128, L * NCH, 2])
    sH = sb("sH", [128, L * NCH])
    axbuf = [sb("axbuf%d" % i, [128, T + 3]) for i in range(2)]
    cvbuf = sb("cvbuf", [128, T + 2])
    tnames = ("t_xc", "t_r", "t_i", "t_a", "t_m", "t_u", "t_h", "t_gl", "t_v1", "rstd")
    tm = {n: sb(n, [128, T]) for n in tnames}
    for a_, c_ in R.alias.items():
        if a_.startswith("t_"):
            tm[a_] = tm[c_]
    xtok = [tm["t_r"], tm["t_i"]]
    xcb = sb("xcb", [128, T], BF16)
    txc = [tm["t_xc"], sb("t_xc1", [128, T])]
    tgl = [tm["t_gl"], sb("t_gl1", [128, T])]
    txck = ["t_xc", "t_xc1"]
    tglk = ["t_gl", "t_gl1"]
    lg = sb("lg", [128, 2, 4, 8])
    gate = sb("gate", [128, 2, 4, 8])
    m8 = sb("m8", [128, 8])
    sm = sb("sm", [128, 8])
    sc4 = sb("sc4", [128, 4])
    dg = [tm["t_v1"], tm["rstd"]]
    R.alias["dg0"] = "t_v1"
    R.alias["dg1"] = "rstd"
    ps = st.enter_context(nc.psum_tensor("ps", [128, 8, 512], F32))

    bank_ctr = [0]

    def nbank():
        b = bank_ctr[0] % 8
        bank_ctr[0] += 1
        if R.last_w.get(("ps", b)) is not None and not R.readers.get(("ps", b)):
            raise RuntimeError("PSUM bank %d re-allocated before its evacuation was recorded" % b)
        return b

    slot_ctr = [0]

    def wtile(src, kcn, nw):
        s = slot_ctr[0] % NSLOT
        slot_ctr[0] += 1
        view = wsl[s][:, 0:kcn * nw].rearrange("p (k n) -> p k n", n=nw)
        srcv = src.rearrange("(k p) n -> p k n", p=128)
        R.dma("gpsimd", lambda e: e.dma_start(out=view, in_=srcv), "w%d" % s, writes=[("ws", s)])
        return view, ("ws", s)

    def mmg(bank, pairs, reads, ncols=512, c0=0):
        n = len(pairs)

        def fn(e):
            ins = None
            for i, (l, r) in enumerate(pairs):
                ins = e.matmul(ps[:, bank, c0:c0 + ncols], lhsT=l, rhs=r, start=(i == 0), stop=(i == n - 1))
            return ins
        return R.op("tensor", fn, reads=reads, writes=[("ps", bank)])

    def act(out, in_, func, reads, writes, bias=None, scale=None):
        kw = {}
        if bias is not None:
            kw["bias"] = bias
        if scale is not None:
            kw["scale"] = scale
        return R.op("scalar", lambda e: e.activation(out=out, in_=in_, func=func, **kw), reads=reads, writes=writes)

    def vtt(out, in0, in1, op, reads, writes):
        return R.op("vector", lambda e: e.tensor_tensor(out=out, in0=in0, in1=in1, op=op), reads=reads, writes=writes)

    def vts(out, in0, s1, s2, op0, op1, reads, writes):
        if s2 is None:
            return R.op("vector", lambda e: e.tensor_scalar(out=out, in0=in0, scalar1=s1, scalar2=None, op0=op0),
                        reads=reads, writes=writes)
        return R.op("vector", lambda e: e.tensor_scalar(out=out, in0=in0, scalar1=s1, scalar2=s2, op0=op0, op1=op1),
                    reads=reads, writes=writes)

    def vstt(out, in0, scalar, in1, op0, op1, reads, writes):
        return R.op("vector", lambda e: e.scalar_tensor_tensor(out=out, in0=in0, scalar=scalar, in1=in1, op0=op0, op1=op1),
                    reads=reads, writes=writes)

    def col(name, i):
        o = PO[name] + i
        return ppt[:, o:o + 1]

    R.dma("sync", lambda e: e.dma_start(out=ppt[:], in_=pp_d[:, :]), "pp", writes=["ppt"])
    R.dma("sync", lambda e: e.dma_start(out=ident_f[:], in_=id_d[:, :]), "id", writes=["ident"])
    R.op("vector", lambda e: e.memset(ones_f[:], 1.0), writes=["ones_f"])
    R.op("vector", lambda e: e.memset(ones_b[:], 1.0), writes=["ones_b"])
    R.op("vector", lambda e: e.memset(cst[:, 0:1], EPS), writes=["cst0"])
    R.op("vector", lambda e: e.memset(cst[:, 1:2], 1.0), writes=["cst"])
    for b in range(NB):
        act(cact[:, :, b], ppt[:, PO["c"] + b * 16:PO["c"] + (b + 1) * 16], AF.Silu, ["ppt"], [("cact", b)])
    lam = ppt[:, PO["lam"]:PO["lam"] + L * 8]
    act(c1t[:], lam, AF.Exp, ["ppt"], ["c1a"], scale=-1.0)
    act(c1t[:], c1t[:], AF.Ln, ["c1a", "cst"], ["c1b"], bias=cst[:, 1:2])
    vts(c1t[:], c1t[:], -8.0, None, ALU.mult, None, ["c1b"], ["c1"])
    def mod_gen(l):
        for jb in range(24 if "mod" not in SKIP else 0):
            w0, k0 = wtile(wmod_d[l * D:l * D + 1024, jb * 512:(jb + 1) * 512], 8, 512)
            w1, k1 = wtile(wmod_d[l * D + 1024:(l + 1) * D, jb * 512:(jb + 1) * 512], 8, 512)
            for j4 in range(4):
                v16 = jb * 4 + j4
                bk = nbank()
                pairs = []
                for kc in range(KC):
                    w = w0 if kc < 8 else w1
                    pairs.append((w[:, kc % 8, j4 * 128:(j4 + 1) * 128], cact[:, kc, :]))
                mmg(bk, pairs, [k0, k1] + [("cact", b) for b in range(NB)], ncols=NB)
                vts(modv[:, :, l * 96 + v16], ps[:, bk, 0:NB], col("bmod", l * 96 + v16), None, ALU.add, None,
                    [("ps", bk), "ppt"], [("modv", l, v16)])
            yield
        for b in range(NB if "mod" not in SKIP else 0):
            for which in range(2):
                gname = "n1g" if which == 0 else "n2g"
                vstt(Amod[:, b, l * 32 + which * 16:l * 32 + which * 16 + 16],
                     modv[:, b, l * 96 + which * 48 + 16:l * 96 + which * 48 + 32], 1.0,
                     ppt[:, PO[gname] + l * 16:PO[gname] + (l + 1) * 16], ALU.add, ALU.mult,
                     [("modv", l, which * 48 + 16 + i) for i in range(16)] + ["ppt"], [("Amod", b, l, which)])
        yield

    for _ in mod_gen(0):
        pass
    bg_gen = [None]

    def modcol(b, l, v, j):
        o = l * 96 + v * 16 + j
        return modv[:, b, o:o + 1]

    def hkeys(par):
        return [("hT", par, kc) for kc in range(KC)]

    def norm_to_hT(b, l, which, par):
        xs, hT = xs2[par], hT2[par]
        for kc in range(KC):
            if kc % 2 == 0:
                act(hT[:, kc, :], xs[:, kc, :], AF.Square, [("xs", par, kc)], [("hT", par, kc)])
            else:
                vtt(hT[:, kc, :], xs[:, kc, :], xs[:, kc, :], ALU.mult, [("xs", par, kc)], [("hT", par, kc)])
        bk = nbank()
        mmg(bk, [(ones_b[:], hT[:, kc, :]) for kc in range(KC)], ["ones_b"] + hkeys(par))
        act(tm["t_s1"][:], ps[:, bk, :], AF.Sqrt, [("ps", bk), "cst0"], ["t_s1"], bias=cst[:, 0:1], scale=1.0 / D)
        R.op("vector", lambda e: e.reciprocal(out=tm["rstd"][:], in_=tm["t_s1"][:]), reads=["t_s1"], writes=["rstd"])
        if which == 2:
            return
        aoff = l * 32 + which * 16
        for kc in range(KC):
            tn = "t_v1" if kc % 2 == 0 else "t_v2"
            vtt(tm[tn][:], xs[:, kc, :], tm["rstd"][:], ALU.mult, [("xs", par, kc), "rstd"], [tn])
            act(hT[:, kc, :], tm[tn][:], AF.Identity, [tn, ("Amod", b, l, which), ("modv", l, which * 48 + kc)],
                [("hT", par, kc)], bias=modcol(b, l, which * 3, kc), scale=Amod[:, b, aoff + kc:aoff + kc + 1])

    def win_tiles(l, c0, nw):
        if nw <= 256:
            w, k = wtile(win_d[l * D:(l + 1) * D, c0:c0 + nw], 16, nw)
            return (lambda kc, a, bb: w[:, kc, a:bb]), [k]
        w0, k0 = wtile(win_d[l * D:l * D + 1024, c0:c0 + nw], 8, nw)
        w1, k1 = wtile(win_d[l * D + 1024:(l + 1) * D, c0:c0 + nw], 8, nw)
        return (lambda kc, a, bb: (w0 if kc < 8 else w1)[:, kc % 8, a:bb]), [k0, k1]

    def mixer(b, l, ti, par, do_norm=True, mid_hook=None):
        xs, hT = xs2[par], hT2[par]
        hT_all = hkeys(par)
        if do_norm:
            norm_to_hT(b, l, 0, par)
        R.dma("gpsimd", lambda e: e.dma_start(out=pwt[:], in_=pw_d[l * 128:(l + 1) * 128, :]), "pw", writes=["pwt"])

        def ZA(c):
            gx, kx = win_tiles(l, c * 128, 128)
            gg, kg = win_tiles(l, DR + c * 128, 128)
            b1 = nbank()
            mmg(b1, [(gx(kc, 0, 128), hT[:, kc, :]) for kc in range(KC)], kx + hT_all)
            b2 = nbank()
            mmg(b2, [(gg(kc, 0, 128), hT[:, kc, :]) for kc in range(KC)], kg + hT_all)
            return b1, b2

        def stage1(c, bax, bag):
            lc = l * NCH + c
            ab = axbuf[c % 2]
            abk = "axbuf%d" % (c % 2)
            xc, xk = txc[c % 2], txck[c % 2]
            if ti == 0:
                R.op("vector", lambda e, ab=ab: e.memset(ab[:, 0:3], 0.0), writes=[abk + "h"])
            else:
                R.op("vector", lambda e, ab=ab, lc=lc: e.tensor_copy(out=ab[:, 0:3], in_=sA[:, lc, :]),
                     reads=[("sA", lc)], writes=[abk + "h"])
            act(ab[:, 3:T + 3], ps[:, bax, :], AF.Copy, [("ps", bax)], [abk])
            act(tgl[c % 2][:], ps[:, bag, :], AF.Gelu_apprx_tanh, [("ps", bag)], [tglk[c % 2]])
            R.op("vector", lambda e, ab=ab, lc=lc: e.tensor_copy(out=sA[:, lc, :], in_=ab[:, T:T + 3]),
                 reads=[abk], writes=[("sA", lc)])
            vts(xc[:], ab[:, 0:T], col("caw", lc * 4 + 0), col("cab", lc), ALU.mult, ALU.add, [abk, abk + "h", "ppt"], [xk])
            for j in range(1, 4):
                vstt(xc[:], ab[:, j:j + T], col("caw", lc * 4 + j), xc[:], ALU.mult, ALU.add,
                     [abk, abk + "h", xk, "ppt"], [xk])
            act(xcb[:], xc[:], AF.Copy, [xk], ["xcb"])
            br = nbank()
            mmg(br, [(pwt[:, c * 128:(c + 1) * 128], xcb[:])], ["pwt", "xcb"])
            bi = nbank()
            mmg(bi, [(pwt[:, (NCH + c) * 128:(NCH + c + 1) * 128], xcb[:])], ["pwt", "xcb"])
            return c, br, bi

        def stage2(c, br, bi):
            lc = l * NCH + c
            xc, xk = txc[c % 2], txck[c % 2]
            act(tm["t_r"][:], ps[:, br, :], AF.Sigmoid, [("ps", br), "ppt"], ["t_r"], bias=col("bra", lc))
            act(tm["t_i"][:], ps[:, bi, :], AF.Sigmoid, [("ps", bi), "ppt"], ["t_i"], bias=col("brx", lc))
            act(tm["t_a"][:], tm["t_r"][:], AF.Exp, ["t_r", "c1"], ["t_a"], scale=c1t[:, lc:lc + 1])
            act(tm["t_m"][:], tm["t_a"][:], AF.Square, ["t_a"], ["t_m"])
            act(tm["t_m"][:], tm["t_m"][:], AF.Sqrt, ["t_m", "cst"], ["t_m"], bias=cst[:, 1:2], scale=-1.0)
            vtt(tm["t_u"][:], tm["t_i"][:], xc[:], ALU.mult, ["t_i", xk], ["t_u"])
            vtt(tm["t_u"][:], tm["t_u"][:], tm["t_m"][:], ALU.mult, ["t_u", "t_m"], ["t_u"])
            if ti == 0:
                R.op("vector", lambda e: e.tensor_tensor_scan(out=tm["t_h"][:], data0=tm["t_a"][:], data1=tm["t_u"][:],
                                                              initial=0.0, op0=ALU.mult, op1=ALU.add),
                     reads=["t_a", "t_u"], writes=["t_h"])
            else:
                R.op("vector", lambda e, lc=lc: e.tensor_tensor_scan(out=tm["t_h"][:], data0=tm["t_a"][:], data1=tm["t_u"][:],
                                                                     initial=sH[:, lc:lc + 1], op0=ALU.mult, op1=ALU.add),
                     reads=["t_a", "t_u", ("sH", lc)], writes=["t_h"])
            R.op("vector", lambda e, lc=lc: e.tensor_copy(out=sH[:, lc:lc + 1], in_=tm["t_h"][:, T - 1:T]),
                 reads=["t_h"], writes=[("sH", lc)])
            vtt(pAB[:, c, :], tgl[c % 2][:], tm["t_h"][:], ALU.mult, [tglk[c % 2], "t_h"], [("pab", c)])

        nxt = ZA(0)
        pend = None
        for c in range(NCH):
            bax, bag = nxt
            if c + 1 < NCH:
                nxt = ZA(c + 1)
            cur = stage1(c, bax, bag)
            if pend is not None:
                stage2(*pend)
            pend = cur
        stage2(*pend)
        if mid_hook is not None:
            mid_hook()

        def ZB(c):
            out = []
            for r in range(3):
                g_, k_ = win_tiles(l, 2 * DR + r * DR + c * 128, 128)
                bk = nbank()
                mmg(bk, [(g_(kc, 0, 128), hT[:, kc, :]) for kc in range(KC)], k_ + hT_all)
                out.append(bk)
            return out

        nxt = ZB(0)
        for c in range(NCH):
            bv, bbg, bcg = nxt
            if c + 1 < NCH:
                nxt = ZB(c + 1)
            lc = l * NCH + c
            if ti == 0:
                R.op("vector", lambda e: e.memset(cvbuf[:, 0:2], 0.0), writes=["cvh"])
            else:
                R.op("vector", lambda e, lc=lc: e.tensor_copy(out=cvbuf[:, 0:2], in_=sBt[:, lc, :]),
                     reads=[("sB", lc)], writes=["cvh"])
            act(tm["t_s1"][:], ps[:, bcg, :], AF.Copy, [("ps", bcg)], ["t_s1"])
            vtt(cvbuf[:, 2:T + 2], tm["t_s1"][:], ps[:, bv, :], ALU.mult, ["t_s1", ("ps", bv)], ["cv"])
            R.op("vector", lambda e, lc=lc: e.tensor_copy(out=sBt[:, lc, :], in_=cvbuf[:, T:T + 2]),
                 reads=["cv"], writes=[("sB", lc)])
            cvo = tm["t_v1"]
            vts(cvo[:], cvbuf[:, 0:T], col("cbw", lc * 3 + 0), col("cbb", lc), ALU.mult, ALU.add, ["cv", "cvh", "ppt"], ["t_v1"])
            for j in range(1, 3):
                vstt(cvo[:], cvbuf[:, j:j + T], col("cbw", lc * 3 + j), cvo[:], ALU.mult, ALU.add,
                     ["cv", "cvh", "t_v1", "ppt"], ["t_v1"])
            vtt(pAB[:, 8 + c, :], cvo[:], ps[:, bbg, :], ALU.mult, ["t_v1", ("ps", bbg)], [("pab", 8 + c)])

        pA_all = [("pab", c) for c in range(NCH)]
        pB_all = [("pab", 8 + c) for c in range(NCH)]
        for jp in range(8):
            cs = slice(jp * 256, (jp + 1) * 256)
            j0, j1 = jp * 2, jp * 2 + 1
            sl0, sl1 = slice(0, 128), slice(128, 256)
            woa, ka = wtile(woa_d[l * DR:(l + 1) * DR, cs], 8, 256)
            bya0 = nbank()
            mmg(bya0, [(woa[:, c, sl0], pAB[:, c, :]) for c in range(NCH)], [ka] + pA_all)
            bya1 = nbank()
            mmg(bya1, [(woa[:, c, sl1], pAB[:, c, :]) for c in range(NCH)], [ka] + pA_all)
            gga, kga = win_tiles(l, 5 * DR + jp * 256, 256)
            bga0 = nbank()
            mmg(bga0, [(gga(kc, 0, 128), hT[:, kc, :]) for kc in range(KC)], kga + hT_all)
            bga1 = nbank()
            mmg(bga1, [(gga(kc, 128, 256), hT[:, kc, :]) for kc in range(KC)], kga + hT_all)
            act(tm["t_r"][:], ps[:, bga0, :], AF.Sigmoid, [("ps", bga0)], ["t_r"])
            vtt(tm["t_a"][:], tm["t_r"][:], ps[:, bya0, :], ALU.mult, ["t_r", ("ps", bya0)], ["t_a"])
            act(tm["t_i"][:], ps[:, bga1, :], AF.Sigmoid, [("ps", bga1)], ["t_i"])
            vtt(tm["t_u"][:], tm["t_i"][:], ps[:, bya1, :], ALU.mult, ["t_i", ("ps", bya1)], ["t_u"])
            wob, kb = wtile(wob_d[l * DR:(l + 1) * DR, cs], 8, 256)
            byb0 = nbank()
            mmg(byb0, [(wob[:, c, sl0], pAB[:, 8 + c, :]) for c in range(NCH)], [kb] + pB_all)
            byb1 = nbank()
            mmg(byb1, [(wob[:, c, sl1], pAB[:, 8 + c, :]) for c in range(NCH)], [kb] + pB_all)
            ggb, kgb = win_tiles(l, 5 * DR + D + jp * 256, 256)
            bgb0 = nbank()
            mmg(bgb0, [(ggb(kc, 0, 128), hT[:, kc, :]) for kc in range(KC)], kgb + hT_all)
            bgb1 = nbank()
            mmg(bgb1, [(ggb(kc, 128, 256), hT[:, kc, :]) for kc in range(KC)], kgb + hT_all)
            act(tm["t_xc"][:], ps[:, bgb0, :], AF.Sigmoid, [("ps", bgb0)], ["t_xc"])
            vtt(tm["t_h"][:], tm["t_xc"][:], ps[:, byb0, :], ALU.mult, ["t_xc", ("ps", byb0)], ["t_h"])
            vtt(mg[:, j0, :], tm["t_a"][:], tm["t_h"][:], ALU.add, ["t_a", "t_h"], [("mg", j0)])
            act(tm["t_gl"][:], ps[:, bgb1, :], AF.Sigmoid, [("ps", bgb1)], ["t_gl"])
            vtt(tm["t_v1"][:], tm["t_gl"][:], ps[:, byb1, :], ALU.mult, ["t_gl", ("ps", byb1)], ["t_v1"])
            vtt(mg[:, j1, :], tm["t_u"][:], tm["t_v1"][:], ALU.add, ["t_u", "t_v1"], [("mg", j1)])
        mg_all = [("mg", j) for j in range(KC)]
        for jg in range(4):
            w0, k0 = wtile(wo_d[l * D:l * D + 1024, jg * 512:(jg + 1) * 512], 8, 512)
            w1, k1 = wtile(wo_d[l * D + 1024:(l + 1) * D, jg * 512:(jg + 1) * 512], 8, 512)
            for j4 in range(4):
                j = jg * 4 + j4
                sl = slice(j4 * 128, (j4 + 1) * 128)
                bo = nbank()
                mmg(bo, [((w0 if k < 8 else w1)[:, k % 8, sl], mg[:, k, :]) for k in range(KC)], [k0, k1] + mg_all)
                vstt(xs[:, j, :], ps[:, bo, :], modcol(b, l, 2, j), xs[:, j, :], ALU.mult, ALU.add,
                     [("ps", bo), ("modv", l, 32 + j), ("xs", par, j)], [("xs", par, j)])

    def emit_gbc(e_):
        buf = e_ % 2
        for par in range(2):
            bk = nbank()
            for s in range(4):
                d_ = dg[s % 2]
                dk = "dg%d" % (s % 2)
                vts(d_[:, 0:128], ident_f[:], gate[:, par, s, e_:e_ + 1], None, ALU.mult, None, ["ident", ("gate", par, s)], [dk])
                R.op("tensor", lambda e, d_=d_, s=s, bk=bk: e.matmul(ps[:, bk, s * 128:(s + 1) * 128], lhsT=ones_f[:], rhs=d_[:, 0:128],
                                                                      start=True, stop=True),
                     reads=[dk, "ones_f"], writes=[("ps", bk)] if s == 0 else [("psx", bk, s)])
            act(gbcj[:, buf, par, :], ps[:, bk, :], AF.Copy, [("ps", bk)] + [("psx", bk, s) for s in range(1, 4)],
                [("gbc", buf, par)])

    def ffn_groups(b, l, groups):
        seen = set()

        def GU(gi):
            gsrc, usrc, dsrc, e_ = groups[gi]
            if e_ is not None and e_ not in seen:
                seen.add(e_)
                emit_gbc(e_)
            for half in range(2):
                gt, kg = wtile(gsrc(half), 16, 256)
                ut, ku = wtile(usrc(half), 16, 256)
                for q2 in range(2):
                    q = half * 2 + q2
                    sl = slice(q2 * 128, (q2 + 1) * 128)
                    for par in range(2):
                        hT = hT2[par]
                        bg = nbank()
                        mmg(bg, [(gt[:, kc, sl], hT[:, kc, :]) for kc in range(KC)], [kg] + hkeys(par))
                        bu = nbank()
                        mmg(bu, [(ut[:, kc, sl], hT[:, kc, :]) for kc in range(KC)], [ku] + hkeys(par))
                        ch = (gi % 2) * 8 + par * 4 + q
                        act(tm["t_s2"][:], ps[:, bg, :], AF.Silu, [("ps", bg)], ["t_s2"])
                        if e_ is not None:
                            vtt(tm["t_s2"][:], tm["t_s2"][:], gbcj[:, e_ % 2, par, :], ALU.mult,
                                ["t_s2", ("gbc", e_ % 2, par)], ["t_s2"])
                        vtt(pAB[:, ch, :], tm["t_s2"][:], ps[:, bu, :], ALU.mult, ["t_s2", ("ps", bu)], [("pab", ch)])

        def DN(gi):
            gsrc, usrc, dsrc, e_ = groups[gi]
            d0, kd0 = wtile(dsrc(0), 4, 1024)
            d1, kd1 = wtile(dsrc(1), 4, 1024)
            for j in range(KC):
                dd, kd = (d0, kd0) if j < 8 else (d1, kd1)
                sl = slice((j % 8) * 128, (j % 8 + 1) * 128)
                for par in range(2):
                    base = (gi % 2) * 8 + par * 4
                    bd = nbank()
                    mmg(bd, [(dd[:, k, sl], pAB[:, base + k, :]) for k in range(4)],
                        [kd] + [("pab", base + k) for k in range(4)])
                    vstt(xs2[par][:, j, :], ps[:, bd, :], modcol(b, l, 5, j), xs2[par][:, j, :], ALU.mult, ALU.add,
                         [("ps", bd), ("modv", l, 80 + j), ("xs", par, j)], [("xs", par, j)])

        GU(0)
        per = -(-26 // len(groups))
        for gi in range(len(groups)):
            if gi + 1 < len(groups):
                GU(gi + 1)
            DN(gi)
            if bg_gen[0] is not None:
                for _ in range(per):
                    if next(bg_gen[0], "end") == "end":
                        bg_gen[0] = None
                        break
        if bg_gen[0] is not None:
            for _ in bg_gen[0]:
                pass
            bg_gen[0] = None

    def ffn_dense(b, l):
        ld = l // 2
        norm_to_hT(b, l, 1, 1)
        groups = []
        for g in range(DFF // 512):
            groups.append((
                lambda half, g=g: wfg_d[ld * D:(ld + 1) * D, g * 512 + half * 256:g * 512 + (half + 1) * 256],
                lambda half, g=g: wfu_d[ld * D:(ld + 1) * D, g * 512 + half * 256:g * 512 + (half + 1) * 256],
                lambda hh, g=g: wfd_d[ld * DFF + g * 512:ld * DFF + (g + 1) * 512, hh * 1024:(hh + 1) * 1024],
                None))
        ffn_groups(b, l, groups)

    def ffn_moe(b, l):
        lm = l // 2
        ro = 2 * NCH * 128
        for par in range(2):
            if par == 1:
                norm_to_hT(b, l, 1, par)
            hT = hT2[par]
            for s in range(4):
                bk = nbank()
                mmg(bk, [(hT[:, kc, s * 128:(s + 1) * 128], pwt[:, ro + kc * 8:ro + (kc + 1) * 8]) for kc in range(KC)],
                    ["pwt"] + hkeys(par), ncols=8)
                gk = ("gate", par, s)
                lk = ("lg", par, s)
                vtt(lg[:, par, s, :], ps[:, bk, 0:8], ppt[:, PO["brt"] + lm * 8:PO["brt"] + (lm + 1) * 8], ALU.add,
                    [("ps", bk), "ppt"], [lk])
                R.op("vector", lambda e, s=s, par=par: e.max(out=m8[:], in_=lg[:, par, s, :]), reads=[lk], writes=["m8"])
                vts(sm[:], lg[:, par, s, :], m8[:, 1:2], None, ALU.is_ge, None, [lk, "m8"], ["sm"])
                vts(sc4[:, 0:1], m8[:, 0:1], -1.0, None, ALU.mult, None, ["m8"], ["m8n"])
                act(gate[:, par, s, :], lg[:, par, s, :], AF.Exp, [lk, "m8n"], [gk], bias=sc4[:, 0:1])
                vtt(gate[:, par, s, :], gate[:, par, s, :], sm[:], ALU.mult, [gk, "sm"], [gk])
                R.op("vector", lambda e, s=s, par=par: e.tensor_reduce(out=sc4[:, 1:2], in_=gate[:, par, s, :], axis=AX.X,
                                                                       op=ALU.add), reads=[gk], writes=["den"])
                R.op("vector", lambda e: e.reciprocal(out=sc4[:, 2:3], in_=sc4[:, 1:2]), reads=["den"], writes=["rden"])
                vts(gate[:, par, s, :], gate[:, par, s, :], sc4[:, 2:3], None, ALU.mult, None, [gk, "rden"], [gk])
        groups = []
        for e_ in range(NE):
            base = (lm * NE + e_) * D
            dbase = (lm * NE + e_) * DFFE
            for g in range(DFFE // 512):
                groups.append((
                    lambda half, g=g, base=base: weg_d[base:base + D, g * 512 + half * 256:g * 512 + (half + 1) * 256],
                    lambda half, g=g, base=base: weu_d[base:base + D, g * 512 + half * 256:g * 512 + (half + 1) * 256],
                    lambda hh, g=g, dbase=dbase: wed_d[dbase + g * 512:dbase + (g + 1) * 512, hh * 1024:(hh + 1) * 1024],
                    e_))
        ffn_groups(b, l, groups)

    n = 0
    for b in range(NB):
        for pair in range(NTILE // 2):
            for par in range(2):
                ti = pair * 2 + par
                r0 = b * S + ti * T
                xs = xs2[par]
                for s in range(4):
                    for q in range(4):
                        xt = xtok[n % 2]
                        xk = "xtok%d" % (n % 2)
                        R.dma("sync", lambda e, xt=xt, s=s, q=q, r0=r0: e.dma_start(
                            out=xt[:], in_=x_d[r0 + s * 128:r0 + (s + 1) * 128, q * 512:(q + 1) * 512]), xk, writes=[xk])
                        bk = nbank()

                        def tp(e, xt=xt, bk=bk):
                            ins = None
                            for i in range(4):
                                ins = e.transpose(out=ps[:, bk, i * 128:(i + 1) * 128], in_=xt[:, i * 128:(i + 1) * 128],
                                                  identity=ident_f[:])
                            return ins
                        R.op("tensor", tp, reads=[xk, "ident"], writes=[("ps", bk)])
                        src3 = ps[:, bk, :].rearrange("p (i c) -> p i c", c=128)
                        dst3 = xs[:, q * 4:(q + 1) * 4, s * 128:(s + 1) * 128]
                        wk = [("xs", par, q * 4 + i) for i in range(4)]
                        if n % 2 == 0:
                            act(dst3, src3, AF.Copy, [("ps", bk)], wk)
                        else:
                            R.op("vector", lambda e, dst3=dst3, src3=src3: e.tensor_copy(out=dst3, in_=src3),
                                 reads=[("ps", bk)], writes=wk)
                        n += 1
            for l in range(L):
                if "mixer" not in SKIP:
                    mixer(b, l, pair * 2, 0, mid_hook=lambda b=b, l=l: norm_to_hT(b, l, 0, 1))
                if "ffn" not in SKIP:
                    norm_to_hT(b, l, 1, 0)
                if "mixer" not in SKIP:
                    mixer(b, l, pair * 2 + 1, 1, do_norm=False)
                if "ffn" in SKIP:
                    continue
                if b == 0 and pair == 0 and l + 1 < L:
                    bg_gen[0] = mod_gen(l + 1)
                if l % 2 == 0:
                    ffn_dense(b, l)
                else:
                    ffn_moe(b, l)
            for par in range(2):
                ti = pair * 2 + par
                r0 = b * S + ti * T
                xs = xs2[par]
                norm_to_hT(b, 0, 2, par)
                for kc in range(KC):
                    vstt(xs[:, kc, :], xs[:, kc, :], col("fg", kc), tm["rstd"][:], ALU.mult, ALU.mult,
                         [("xs", par, kc), "rstd", "ppt"], [("xs", par, kc)])
                for s in range(4):
                    for q in range(4):
                        bk = nbank()

                        def tp2(e, s=s, q=q, bk=bk, xs=xs):
                            ins = None
                            for i in range(4):
                                ins = e.transpose(out=ps[:, bk, i * 128:(i + 1) * 128],
                                                  in_=xs[:, q * 4 + i, s * 128:(s + 1) * 128], identity=ident_f[:])
                            return ins
                        R.op("tensor", tp2, reads=[("xs", par, q * 4 + i) for i in range(4)] + ["ident"], writes=[("ps", bk)])
                        xt = xtok[n % 2]
                        xk = "xtok%d" % (n % 2)
                        n += 1
                        act(xt[:], ps[:, bk, :], AF.Copy, [("ps", bk)], [xk])
                        R.dma("sync", lambda e, xt=xt, s=s, q=q, r0=r0: e.dma_start(
                            out=y_d[r0 + s * 128:r0 + (s + 1) * 128, q * 512:(q + 1) * 512], in_=xt[:]), "o" + xk,
                            reads=[xk], writes=["out_" + xk])
    R.wait_all("sync", ["out_xtok0", "out_xtok1"])
    stats = R.emit(nc, st)
    st.close()
    return nc, stats


def host_pack(inp, cfg, core, ncores):
    NB, S, L = cfg["NB"], cfg["S"], cfg["L"]
    NE = cfg["NE"]
    LM = L // 2
    PO, NPP = pp_layout(NB, L, LM)
    pp = np.zeros((128, NPP), np.float32)
    f = lambda a: np.asarray(a, np.float32)
    c = f(inp["c"])[core * NB:(core + 1) * NB]
    pp[:, PO["c"]:PO["c"] + NB * 16] = c.reshape(NB, 16, 128).transpose(2, 0, 1).reshape(128, NB * 16)
    pp[:, PO["bmod"]:PO["bmod"] + L * 96] = f(inp["b_mod"])[:L].reshape(L, 96, 128).transpose(2, 0, 1).reshape(128, L * 96)
    pp[:, PO["n1g"]:PO["n1g"] + L * 16] = f(inp["norm1_g"])[:L].reshape(L, 16, 128).transpose(2, 0, 1).reshape(128, L * 16)
    pp[:, PO["n2g"]:PO["n2g"] + L * 16] = f(inp["norm2_g"])[:L].reshape(L, 16, 128).transpose(2, 0, 1).reshape(128, L * 16)
    pp[:, PO["fg"]:PO["fg"] + 16] = f(inp["final_g"]).reshape(16, 128).T
    pp[:, PO["caw"]:PO["caw"] + L * 32] = f(inp["conv_a_w"])[:L].reshape(L, 4, 8, 128).transpose(3, 0, 2, 1).reshape(128, L * 32)
    for name, key in (("cab", "conv_a_b"), ("bra", "b_rg_a"), ("brx", "b_rg_x"), ("lam", "rg_lambda"), ("cbb", "conv_b_b")):
        pp[:, PO[name]:PO[name] + L * 8] = f(inp[key])[:L].reshape(L, 8, 128).transpose(2, 0, 1).reshape(128, L * 8)
    pp[:, PO["cbw"]:PO["cbw"] + L * 24] = f(inp["conv_b_w"])[:L].reshape(L, 3, 8, 128).transpose(3, 0, 2, 1).reshape(128, L * 24)
    if LM > 0:
        pp[:, PO["brt"]:PO["brt"] + LM * 8] = np.broadcast_to(f(inp["b_router"])[:LM].reshape(1, LM * 8), (128, LM * 8))
    NPW = 2 * NCH * 128 + KC * 8
    pw = np.zeros((L, 128, NPW), np.float32)
    for gi, key in enumerate(("w_rg_a", "w_rg_x")):
        w = f(inp[key])[:L]
        for c_ in range(NCH):
            for hh in range(2):
                pw[:, hh * 64:(hh + 1) * 64, (gi * NCH + c_) * 128 + hh * 64:(gi * NCH + c_) * 128 + (hh + 1) * 64] = w[:, 2 * c_ + hh]
    for l in range(L):
        if l % 2 == 1:
            wr = f(inp["w_router"])[l // 2]
            pw[l, :, 2 * NCH * 128:] = wr.reshape(16, 128, NE).transpose(1, 0, 2).reshape(128, 16 * NE)
    return pp, pw.reshape(L * 128, NPW)


FULL = dict(NB=1, S=2048, L=4, DFF=6144, DFFE=3072, NE=8)
NCORES = 8
_cache = {}


def run(inp, cfg, ncores):
    NB, S, L = cfg["NB"], cfg["S"], cfg["L"]
    LD, LM = (L + 1) // 2, L // 2
    key = tuple(sorted((k, v) for k, v in cfg.items()))
    if key not in _cache:
        _cache[key] = build(cfg)
    nc, stats = _cache[key]
    f = lambda a: np.ascontiguousarray(np.asarray(a, np.float32))
    shared = {
        "ident": np.eye(128, dtype=np.float32),
        "w_mod": f(inp["w_mod"])[:L].reshape(L * D, 6 * D),
        "w_in": f(inp["w_in"])[:L].reshape(L * D, DIN),
        "w_out_a": f(inp["w_out_a"])[:L].reshape(L * DR, D),
        "w_out_b": f(inp["w_out_b"])[:L].reshape(L * DR, D),
        "w_o": f(inp["w_o"])[:L].reshape(L * D, D),
        "w_ff_gate": f(inp["w_ff_gate"])[:LD].reshape(LD * D, cfg["DFF"]),
        "w_ff_up": f(inp["w_ff_up"])[:LD].reshape(LD * D, cfg["DFF"]),
        "w_ff_down": f(inp["w_ff_down"])[:LD].reshape(LD * cfg["DFF"], D),
        "w_e_gate": f(inp["w_e_gate"])[:max(LM, 1)].reshape(-1, cfg["DFFE"]),
        "w_e_up": f(inp["w_e_up"])[:max(LM, 1)].reshape(-1, cfg["DFFE"]),
        "w_e_down": f(inp["w_e_down"])[:max(LM, 1)].reshape(-1, D),
    }
    x = f(inp["x"])
    in_maps = []
    for core in range(ncores):
        pp, pw = host_pack(inp, cfg, core, ncores)
        m = dict(shared)
        m["x"] = x[core * NB:(core + 1) * NB].reshape(NB * S, D)
        m["pp"] = pp
        m["pw"] = pw
        in_maps.append(m)
    res = run_bass_kernel_spmd(nc, in_maps, core_ids=list(range(ncores)))
    out = np.concatenate([r["y"].reshape(NB, S, D) for r in res.results], axis=0)
    return out.astype(np.float32)


def kernel(**inputs):
    return run(inputs, FULL, NCORES)
```
